# Optimizing a Trainium2 kernel written in Bass

```python
import math
import jax
import jax.numpy as jnp
from jax import lax
import numpy as np

D_MODEL = 1024
BATCH = 1
SEQ = 16384
DEPTH = 1
DEC_BATCH = 128
DEC_SEQ = 4
PAST_LEN = 16384
PAGE_SIZE = 128

HEAD_DIM = 64
A_HEADS = 8
A_KV_HEADS = 2
A_WINDOW = 128
B_KV_HEADS = 4
B_GROUPS = ((128, 1), (512, 4), (2048, 16))
B_HEADS = B_KV_HEADS * len(B_GROUPS)
B_WINDOW_MAX = max(w for w, _ in B_GROUPS)
B_QBLOCK = 128
MEM_TOKENS = 256
M_HEADS = 4
M_HEAD_DIM = 128
REL_BUCKETS = 32
REL_MAX_EXACT = REL_BUCKETS // 2
REL_MAX_DISTANCE = B_WINDOW_MAX
REL_HEADS = A_HEADS + B_HEADS
N_BRANCHES = 3
IN_SPLITS = (A_HEADS * HEAD_DIM, A_KV_HEADS * HEAD_DIM, A_KV_HEADS * HEAD_DIM,
             B_HEADS * HEAD_DIM, B_KV_HEADS * HEAD_DIM, B_KV_HEADS * HEAD_DIM,
             M_HEADS * M_HEAD_DIM, N_BRANCHES * D_MODEL)
IN_OFFSETS = tuple(int(o) for o in np.cumsum(IN_SPLITS)[:-1])
IN_WIDTH = sum(IN_SPLITS)
N_EXPERTS = 32
TOP_K = 4
D_FF = D_MODEL
SWIGLU_LIMIT = 7.0
SWIGLU_ALPHA = 1.702
EXPERT_BLOCK = 128
LN_EPS = 1e-5
DEEPNORM_ALPHA = (2 * DEPTH) ** 0.25
DEEPNORM_BETA = (8 * DEPTH) ** -0.25

kernel_name = 'hybrid_swa_dilated_memory_moe_step'


def layer_norm(x, g, b):
    xf = x.astype(jnp.float32)
    mu = xf.mean(-1, keepdims=True)
    var = jnp.square(xf - mu).mean(-1, keepdims=True)
    y = (xf - mu) * lax.rsqrt(var + LN_EPS) * g.astype(jnp.float32) + b.astype(jnp.float32)
    return y.astype(x.dtype)


def t5_bucket(dist):
    d = jnp.maximum(dist, 0)
    d_large = jnp.maximum(d, REL_MAX_EXACT).astype(jnp.float32)
    large = REL_MAX_EXACT + (jnp.log(d_large / REL_MAX_EXACT) / math.log(REL_MAX_DISTANCE / REL_MAX_EXACT)
                             * (REL_BUCKETS - REL_MAX_EXACT)).astype(jnp.int32)
    return jnp.where(d < REL_MAX_EXACT, d, jnp.minimum(large, REL_BUCKETS - 1))


def split_projection(x, w_in):
    bsz, n = x.shape[:2]
    qa, ka, va, qb, kb, vb, qm, gates = jnp.split(x @ w_in, IN_OFFSETS, axis=-1)
    return (qa.reshape(bsz, n, A_HEADS, HEAD_DIM), ka.reshape(bsz, n, A_KV_HEADS, HEAD_DIM),
            va.reshape(bsz, n, A_KV_HEADS, HEAD_DIM), qb.reshape(bsz, n, B_HEADS, HEAD_DIM),
            kb.reshape(bsz, n, B_KV_HEADS, HEAD_DIM), vb.reshape(bsz, n, B_KV_HEADS, HEAD_DIM),
            qm.reshape(bsz, n, M_HEADS, M_HEAD_DIM), gates)


def sink_window_attention(q, k, v, dist, valid, rel_bias, sinks):
    bsz, nblk, qlen = q.shape[:3]
    rep = A_HEADS // A_KV_HEADS
    qg = q.reshape(bsz, nblk, qlen, A_KV_HEADS, rep, HEAD_DIM)
    s = jnp.einsum('bnqgrd,bnkgd->bngrqk', qg, k).astype(jnp.float32) * HEAD_DIM ** -0.5
    bias = rel_bias[:, :A_HEADS][t5_bucket(dist)].astype(jnp.float32)
    s = s + jnp.moveaxis(bias, -1, 0).reshape(A_KV_HEADS, rep, *dist.shape)
    s = jnp.where(valid[:, None, None], s, -jnp.inf)
    sink = sinks.astype(jnp.float32).reshape(A_KV_HEADS, rep, 1, 1)
    m = jnp.maximum(s.max(-1, keepdims=True), sink)
    p = jnp.exp(s - m)
    p = p / (p.sum(-1, keepdims=True) + jnp.exp(sink - m))
    o = jnp.einsum('bngrqk,bnkgd->bnqgrd', p.astype(v.dtype), v)
    return o.reshape(bsz, nblk, qlen, A_HEADS * HEAD_DIM)


def dilated_attention(q, k, v, qpos, rel_bias):
    outs, lses = [], []
    for g, (window, dil) in enumerate(B_GROUPS):
        dist = dil * jnp.arange(window // dil + 1, dtype=jnp.int32)
        idx = qpos[:, None] - dist[None, :]
        valid = idx >= 0
        kg = jnp.take(k, jnp.maximum(idx, 0), axis=1)
        vg = jnp.take(v, jnp.maximum(idx, 0), axis=1)
        qg = q[:, :, g * B_KV_HEADS:(g + 1) * B_KV_HEADS]
        lo = A_HEADS + g * B_KV_HEADS
        bias = rel_bias[:, lo:lo + B_KV_HEADS][t5_bucket(dist)].astype(jnp.float32).T
        s = jnp.einsum('bqhd,bqjhd->bqhj', qg, kg).astype(jnp.float32) * HEAD_DIM ** -0.5 + bias
        s = jnp.where(valid[None, :, None, :], s, -jnp.inf)
        lse = jax.nn.logsumexp(s, axis=-1)
        p = jnp.exp(s - lse[..., None])
        outs.append(jnp.einsum('bqhj,bqjhd->bqhd', p.astype(v.dtype), vg).astype(jnp.float32))
        lses.append(lse)
    w = jax.nn.softmax(jnp.stack(lses), axis=0)
    o = (w[..., None] * jnp.stack(outs)).sum(0)
    return o.reshape(q.shape[0], q.shape[1], B_KV_HEADS * HEAD_DIM).astype(q.dtype)


def memory_kv(mem, w_mem_kv):
    bsz, n = mem.shape[:2]
    kv = (mem @ w_mem_kv).reshape(bsz, n, 2, M_HEADS, M_HEAD_DIM)
    return kv[:, :, 0], kv[:, :, 1]


def memory_attention(qm, mk, mv):
    s = jnp.einsum('bthd,bmhd->bhtm', qm, mk).astype(jnp.float32) * M_HEAD_DIM ** -0.5
    p = jax.nn.softmax(s, axis=-1)
    o = jnp.einsum('bhtm,bmhd->bthd', p.astype(mv.dtype), mv)
    return o.reshape(qm.shape[0], qm.shape[1], M_HEADS * M_HEAD_DIM)


def merge_branches(oa, ob, om, gates, w_br_a, w_br_b, w_br_m, w_o):
    g = jax.nn.sigmoid(gates.astype(jnp.float32)).astype(oa.dtype)
    ga, gb, gm = jnp.split(g, N_BRANCHES, axis=-1)
    u = ga * (oa @ w_br_a) + gb * (ob @ w_br_b) + gm * (om @ w_br_m)
    return u @ w_o


def moe_ffn(x, w_router, b_router, w_gu, b_gu, w_down, b_down):
    lead = x.shape[:-1]
    xt = x.reshape(-1, D_MODEL)
    n_tok = xt.shape[0]
    logits = (xt @ w_router).astype(jnp.float32) + b_router.astype(jnp.float32)
    top_val, top_idx = lax.top_k(logits, TOP_K)
    gate = jax.nn.softmax(top_val, axis=-1)
    n_assign = n_tok * TOP_K
    flat_e = top_idx.reshape(-1)
    order = jnp.argsort(flat_e)
    sorted_e = flat_e[order]
    counts = jnp.bincount(flat_e, length=N_EXPERTS)
    padded = (counts + EXPERT_BLOCK - 1) // EXPERT_BLOCK * EXPERT_BLOCK
    grp_start = jnp.cumsum(counts) - counts
    pad_end = jnp.cumsum(padded)
    pad_start = pad_end - padded
    dest = pad_start[sorted_e] + jnp.arange(n_assign) - grp_start[sorted_e]
    n_blocks = -(-n_assign // EXPERT_BLOCK) + N_EXPERTS
    n_rows = n_blocks * EXPERT_BLOCK
    row_tok = jnp.full((n_rows,), n_tok, jnp.int32).at[dest].set((order // TOP_K).astype(jnp.int32))
    row_gate = jnp.zeros((n_rows,), jnp.float32).at[dest].set(gate.reshape(-1)[order])
    block_expert = jnp.minimum(
        jnp.searchsorted(pad_end, jnp.arange(n_blocks) * EXPERT_BLOCK, side='right'), N_EXPERTS - 1)
    x_rows = jnp.concatenate([xt, jnp.zeros((1, D_MODEL), xt.dtype)])[row_tok]
    x_rows = x_rows.reshape(n_blocks, EXPERT_BLOCK, D_MODEL)

    def expert_block(args):
        xb, e = args
        gu = xb @ w_gu[e] + b_gu[e]
        g = jnp.minimum(gu[:, :D_FF], SWIGLU_LIMIT)
        u = jnp.clip(gu[:, D_FF:], -SWIGLU_LIMIT, SWIGLU_LIMIT)
        h = (u + 1.0) * g * jax.nn.sigmoid(SWIGLU_ALPHA * g)
        return h @ w_down[e] + b_down[e]

    y_rows = lax.map(expert_block, (x_rows, block_expert)).reshape(n_rows, D_MODEL)
    y = jnp.zeros((n_tok + 1, D_MODEL), y_rows.dtype).at[row_tok].add(
        y_rows * row_gate[:, None].astype(y_rows.dtype))
    return y[:n_tok].reshape(*lead, D_MODEL)


def finish_layer(x, mixed, ln1_g, ln1_b, ln2_g, ln2_b, w_router, b_router, w_gu, b_gu, w_down, b_down):
    h = layer_norm(DEEPNORM_ALPHA * x + mixed, ln1_g, ln1_b)
    f = moe_ffn(h, w_router, b_router, w_gu, b_gu, w_down, b_down)
    return layer_norm(DEEPNORM_ALPHA * h + f, ln2_g, ln2_b)


def prompt_layer(x, mem, rel_bias, params):
    (sinks_a, w_in, w_mem_kv, w_br_a, w_br_b, w_br_m, w_o, ln1_g, ln1_b, ln2_g, ln2_b,
     w_router, b_router, w_gu, b_gu, w_down, b_down) = params
    bsz, seq, _ = x.shape
    qa, ka, va, qb, kb, vb, qm, gates = split_projection(x, w_in)
    nblk = seq // A_WINDOW

    def band(t):
        t = t.reshape(bsz, nblk, A_WINDOW, t.shape[2], HEAD_DIM)
        prev = jnp.pad(t[:, :-1], ((0, 0), (1, 0), (0, 0), (0, 0), (0, 0)))
        return jnp.concatenate([prev, t], axis=2)

    qi = jnp.arange(A_WINDOW)[:, None]
    kj = jnp.arange(2 * A_WINDOW)[None, :]
    dist = A_WINDOW + qi - kj
    valid = (dist >= 0) & (dist < A_WINDOW) & ((jnp.arange(nblk)[:, None, None] > 0) | (kj >= A_WINDOW))
    oa = sink_window_attention(qa.reshape(bsz, nblk, A_WINDOW, A_HEADS, HEAD_DIM), band(ka), band(va),
                               dist, valid, rel_bias, sinks_a).reshape(bsz, seq, A_HEADS * HEAD_DIM)
    nq = seq // B_QBLOCK
    qpos = jnp.arange(seq, dtype=jnp.int32).reshape(nq, B_QBLOCK)
    qblocks = jnp.moveaxis(qb.reshape(bsz, nq, B_QBLOCK, B_HEADS, HEAD_DIM), 1, 0)
    ob = lax.map(lambda a: dilated_attention(a[0], kb, vb, a[1], rel_bias), (qblocks, qpos))
    ob = jnp.moveaxis(ob, 0, 1).reshape(bsz, seq, B_KV_HEADS * HEAD_DIM)
    mk, mv = memory_kv(mem, w_mem_kv)
    om = memory_attention(qm, mk, mv)
    mixed = merge_branches(oa, ob, om, gates, w_br_a, w_br_b, w_br_m, w_o)
    y = finish_layer(x, mixed, ln1_g, ln1_b, ln2_g, ln2_b, w_router, b_router, w_gu, b_gu, w_down, b_down)
    la = min(A_WINDOW, seq)
    lb = min(B_WINDOW_MAX, seq)
    return y, ka[:, seq - la:], va[:, seq - la:], kb[:, seq - lb:], vb[:, seq - lb:], mk, mv


def sample_layer(x, ca_k, ca_v, cb_k, cb_v, cm_k, cm_v, rel_bias, params):
    (sinks_a, w_in, w_mem_kv, w_br_a, w_br_b, w_br_m, w_o, ln1_g, ln1_b, ln2_g, ln2_b,
     w_router, b_router, w_gu, b_gu, w_down, b_down) = params
    bsz, n_new, _ = x.shape
    qa, ka, va, qb, kb, vb, qm, gates = split_projection(x, w_in)
    la = ca_k.shape[1]
    keys_a = jnp.concatenate([ca_k, ka], axis=1)[:, None]
    vals_a = jnp.concatenate([ca_v, va], axis=1)[:, None]
    qi = jnp.arange(n_new)[:, None]
    kj = jnp.arange(la + n_new)[None, :]
    dist = la + qi - kj
    valid = ((dist >= 0) & (dist < A_WINDOW))[None]
    oa = sink_window_attention(qa[:, None], keys_a, vals_a, dist, valid, rel_bias, sinks_a)[:, 0]
    lb = cb_k.shape[1]
    ob = dilated_attention(qb, jnp.concatenate([cb_k, kb], axis=1), jnp.concatenate([cb_v, vb], axis=1),
                           lb + jnp.arange(n_new, dtype=jnp.int32), rel_bias)
    om = memory_attention(qm, cm_k, cm_v)
    mixed = merge_branches(oa, ob, om, gates, w_br_a, w_br_b, w_br_m, w_o)
    y = finish_layer(x, mixed, ln1_g, ln1_b, ln2_g, ln2_b, w_router, b_router, w_gu, b_gu, w_down, b_down)
    return y, ka, va, kb, vb


def setup_inputs(seed: int = 0) -> dict:
    key = jax.random.key(seed)
    ks = jax.random.split(key, 32)
    f32 = jnp.float32
    beta = DEEPNORM_BETA
    la = min(A_WINDOW, PAST_LEN)
    lb = min(B_WINDOW_MAX, PAST_LEN)

    def nrm(i, shape, scale):
        return jax.random.normal(ks[i], shape, f32) * scale

    col_scales = (1.0, 1.0, beta, 1.0, 1.0, beta, 1.0, 1.0)
    in_scale = jnp.concatenate([jnp.full((w,), s, f32) for w, s in zip(IN_SPLITS, col_scales)]) * D_MODEL ** -0.5
    mw = M_HEADS * M_HEAD_DIM
    mem_scale = jnp.concatenate([jnp.ones((mw,), f32), jnp.full((mw,), beta, f32)]) * D_MODEL ** -0.5
    return {
        'x_prompt': nrm(0, (BATCH, SEQ, D_MODEL), 1.0),
        'x_sample': nrm(1, (DEC_BATCH, DEC_SEQ, D_MODEL), 1.0),
        'cache_a_k': nrm(2, (DEPTH, DEC_BATCH, la, A_KV_HEADS, HEAD_DIM), 1.0),
        'cache_a_v': nrm(3, (DEPTH, DEC_BATCH, la, A_KV_HEADS, HEAD_DIM), beta),
        'cache_b_k': nrm(4, (DEPTH, DEC_BATCH, lb, B_KV_HEADS, HEAD_DIM), 1.0),
        'cache_b_v': nrm(5, (DEPTH, DEC_BATCH, lb, B_KV_HEADS, HEAD_DIM), beta),
        'cache_mem_k': nrm(6, (DEPTH, DEC_BATCH, MEM_TOKENS, M_HEADS, M_HEAD_DIM), 1.0),
        'cache_mem_v': nrm(7, (DEPTH, DEC_BATCH, MEM_TOKENS, M_HEADS, M_HEAD_DIM), beta),
        'mem_prompt': nrm(8, (BATCH, MEM_TOKENS, D_MODEL), 1.0),
        'rel_bias': nrm(9, (REL_BUCKETS, REL_HEADS), 0.2),
        'sinks_a': nrm(10, (DEPTH, A_HEADS), 0.5),
        'w_in': nrm(11, (DEPTH, D_MODEL, IN_WIDTH), 1.0) * in_scale,
        'w_mem_kv': nrm(12, (DEPTH, D_MODEL, 2 * mw), 1.0) * mem_scale,
        'w_br_a': nrm(13, (DEPTH, A_HEADS * HEAD_DIM, D_MODEL), beta * (A_HEADS * HEAD_DIM) ** -0.5),
        'w_br_b': nrm(14, (DEPTH, B_KV_HEADS * HEAD_DIM, D_MODEL), beta * (B_KV_HEADS * HEAD_DIM) ** -0.5),
        'w_br_m': nrm(15, (DEPTH, mw, D_MODEL), beta * mw ** -0.5),
        'w_o': nrm(16, (DEPTH, D_MODEL, D_MODEL), beta * D_MODEL ** -0.5),
        'ln1_g': 1.0 + nrm(17, (DEPTH, D_MODEL), 0.05),
        'ln1_b': nrm(18, (DEPTH, D_MODEL), 0.05),
        'ln2_g': 1.0 + nrm(19, (DEPTH, D_MODEL), 0.05),
        'ln2_b': nrm(20, (DEPTH, D_MODEL), 0.05),
        'w_router': nrm(21, (DEPTH, D_MODEL, N_EXPERTS), D_MODEL ** -0.5),
        'b_router': nrm(22, (DEPTH, N_EXPERTS), 0.01),
        'w_gu': nrm(23, (DEPTH, N_EXPERTS, D_MODEL, 2 * D_FF), beta * D_MODEL ** -0.5),
        'b_gu': nrm(24, (DEPTH, N_EXPERTS, 2 * D_FF), 0.02),
        'w_down': nrm(25, (DEPTH, N_EXPERTS, D_FF, D_MODEL), beta * D_FF ** -0.5),
        'b_down': nrm(26, (DEPTH, N_EXPERTS, D_MODEL), 0.02),
    }


def reference(x_prompt, x_sample, cache_a_k, cache_a_v, cache_b_k, cache_b_v, cache_mem_k, cache_mem_v,
              mem_prompt, rel_bias, sinks_a, w_in, w_mem_kv, w_br_a, w_br_b, w_br_m, w_o,
              ln1_g, ln1_b, ln2_g, ln2_b, w_router, b_router, w_gu, b_gu, w_down, b_down):
    y_prompt, y_sample = x_prompt, x_sample
    pa_k, pa_v, pb_k, pb_v, pm_k, pm_v = [], [], [], [], [], []
    sa_k, sa_v, sb_k, sb_v = [], [], [], []
    for l in range(DEPTH):
        params = (sinks_a[l], w_in[l], w_mem_kv[l], w_br_a[l], w_br_b[l], w_br_m[l], w_o[l],
                  ln1_g[l], ln1_b[l], ln2_g[l], ln2_b[l], w_router[l], b_router[l],
                  w_gu[l], b_gu[l], w_down[l], b_down[l])
        y_prompt, a_k, a_v, b_k, b_v, m_k, m_v = prompt_layer(y_prompt, mem_prompt, rel_bias, params)
        pa_k.append(a_k)
        pa_v.append(a_v)
        pb_k.append(b_k)
        pb_v.append(b_v)
        pm_k.append(m_k)
        pm_v.append(m_v)
        y_sample, a_k, a_v, b_k, b_v = sample_layer(y_sample, cache_a_k[l], cache_a_v[l], cache_b_k[l],
                                                     cache_b_v[l], cache_mem_k[l], cache_mem_v[l],
                                                     rel_bias, params)
        sa_k.append(a_k)
        sa_v.append(a_v)
        sb_k.append(b_k)
        sb_v.append(b_v)
    return (y_prompt, y_sample,
            jnp.stack(pa_k), jnp.stack(pa_v), jnp.stack(pb_k), jnp.stack(pb_v), jnp.stack(pm_k), jnp.stack(pm_v),
            jnp.stack(sa_k), jnp.stack(sa_v), jnp.stack(sb_k), jnp.stack(sb_v))
```

```python
import math
from contextlib import ExitStack
import numpy as np
import concourse.bass as bass
import concourse.mybir as mybir
from concourse.bass_utils import run_bass_kernel_spmd

F32 = mybir.dt.float32
BF16 = mybir.dt.bfloat16
I32 = mybir.dt.int32
ALU = mybir.AluOpType
AF = mybir.ActivationFunctionType
AX = mybir.AxisListType


class Buf:
    __slots__ = ("name", "w", "r", "dsem", "dval")

    def __init__(self, name):
        self.name = name
        self.w = None
        self.r = {}
        self.dsem = None
        self.dval = 0


class Ctx:
    def __init__(self, nc):
        self.nc = nc
        self.engs = {"pe": nc.tensor, "act": nc.scalar, "dve": nc.vector,
                     "pool": nc.gpsimd, "sp": nc.sync}
        self.esem = {k: nc.alloc_semaphore("es_" + k) for k in ("pe", "act", "dve", "pool")}
        self.ecnt = {k: 0 for k in self.esem}
        self.seen = {k: {} for k in self.engs}
        self.nbuf = 0
        self.skip_own = {"pe"}
        self.all_dma = []
        self.dma_bufs = []

    def buf(self, name=None):
        self.nbuf += 1
        return Buf(name or ("b%d" % self.nbuf))

    def bufs(self, n, name="b"):
        return [self.buf("%s%d" % (name, i)) for i in range(n)]

    def _wait(self, e, sem, val):
        if sem is None:
            return
        seen = self.seen[e]
        if seen.get(sem, 0) >= val:
            return
        if e in self.skip_own and sem is self.esem.get(e):
            return
        self.engs[e].wait_ge(sem, val)
        seen[sem] = val

    def _deps(self, e, reads, writes):
        own = self.esem.get(e)
        for b in reads:
            if b.w is not None:
                self._wait(e, *b.w)
        for b in writes:
            if b.w is not None:
                self._wait(e, *b.w)
            for s, v in b.r.items():
                self._wait(e, s, v)

    def op(self, e, fn, reads=(), writes=(), inc=True):
        self._deps(e, reads, writes)
        ins = fn(self.engs[e])
        sem = self.esem[e]
        if inc:
            self.ecnt[e] += 1
            v = self.ecnt[e]
            ins.then_inc(sem, 1)
        else:
            v = self.ecnt[e] + 1
        for b in reads:
            if b.r.get(sem, 0) < v:
                b.r[sem] = v
        for b in writes:
            b.w = (sem, v)
            b.r = {}
        return ins

    def dma(self, q, fn, reads=(), writes=(), own=None, final=False):
        self._deps(q, reads, writes)
        if own is None:
            own = writes[0] if writes else reads[0]
        if own.dsem is None:
            own.dsem = self.nc.alloc_semaphore("ds_" + own.name)
            self.dma_bufs.append(own)
        ins = fn(self.engs[q])
        own.dval += 16
        ins.then_inc(own.dsem, 16)
        ev = (own.dsem, own.dval)
        for b in reads:
            if b.r.get(ev[0], 0) < ev[1]:
                b.r[ev[0]] = ev[1]
        for b in writes:
            b.w = ev
            b.r = {}
        if final:
            self.all_dma.append(ev)
        return ins

    def barrier(self, engines=("pe", "act", "dve", "pool", "sp")):
        for e in engines:
            for k, sem in self.esem.items():
                if k != e and self.ecnt[k] > 0:
                    self._wait(e, sem, self.ecnt[k])
            for b in self.dma_bufs:
                if b.dval > 0:
                    self._wait(e, b.dsem, b.dval)

    def finish(self, e="sp"):
        for sem, val in self.all_dma:
            self._wait(e, sem, val)


NT = 2048
NS = 64
NTOK = NT + NS
NB = 16
ALPHA = float(2.0 ** 0.25)
NEG = -30000.0
LN_EPS = 1e-5
NH_G = (1, 4, 16)
FVL = 384
FVR = 129
DEBUG_OUT = False
CAP = 384
NRX = 32 * CAP


def build(n_experts=32, stop=None):
    nc = bass.Bass("TRN2", target_bir_lowering=False)
    c = Ctx(nc)

    def din(n, s, dt=F32):
        return nc.dram_tensor(n, list(s), dt, kind="ExternalInput").ap()

    def dout(n, s, dt=F32):
        return nc.dram_tensor(n, list(s), dt, kind="ExternalOutput").ap()

    xt = [din("xt0", [1024, 17 * 128]), din("xt1", [1024, 20 * 128]), din("xt2", [1024, 32 * 128])]
    xtok = din("xtok", [NT, 1024])
    xst = din("xst", [1024, NS])
    xs = din("xs", [NS, 1024])
    memT = din("memT", [1024, 256])
    cakT = din("cakT", [64, NB, 2, 128])
    cav = din("cav", [128, NB, 128])
    cbkT = din("cbkT", [NB, 64, 9 * 4 * 128])
    cbv = din("cbv", [NB, 128, 9 * 4 * 64])
    cmkT = din("cmkT", [NB, 128, 4 * 256])
    cmv = din("cmv", [NB, 128, 2 * 512])
    rel_bias = din("rel_bias", [32, 20])
    sinks = din("sinks", [1, 8])
    oh = din("oh", [33, 4, FVL])
    hmask = din("hmask", [128, 1])
    ident = din("ident", [128, 128])
    w_in = din("w_in", [1024, 5632])
    w_mem_kv = din("w_mem_kv", [1024, 1024])
    w_br_a = din("w_br_a", [512, 1024])
    w_br_b = din("w_br_b", [256, 1024])
    w_br_m = din("w_br_m", [512, 1024])
    w_o = din("w_o", [1024, 1024])
    ln1_g = din("ln1_g", [1, 1024]); ln1_b = din("ln1_b", [1, 1024])
    ln2_g = din("ln2_g", [1, 1024]); ln2_b = din("ln2_b", [1, 1024])
    w_router = din("w_router", [1024, 32]); b_router = din("b_router", [1, 32])
    w_gu = din("w_gu", [32, 4, 128, 4096]); b_gu = din("b_gu", [128, 32, 16])
    w_down = din("w_down", [32, 2, 128, 4096]); b_down = din("b_down", [32, 1024])

    y_o = dout("y", [NT, 1024]); ys_o = dout("ys", [NS, 1024])
    ak_o = dout("ak", [128, 128]); av_o = dout("av", [128, 128])
    bk_o = dout("bk", [NT, 256]); bv_o = dout("bv", [NT, 256])
    mk_o = dout("mk", [256, 512]); mv_o = dout("mv", [256, 512])
    sak_o = dout("sak", [NS, 128]); sav_o = dout("sav", [NS, 128])
    sbk_o = dout("sbk", [NS, 256]); sbv_o = dout("sbv", [NS, 256])

    ltri_d = din("ltri", [128, 128]); iota_d = din("iota_e", [128, 32]); ecs_d = din("ecs", [128, 32])
    Xd = nc.dram_tensor("Xd", [NRX, 1024], BF16, kind="Internal"); b_Xd = c.buf("Xd")
    Yd = nc.dram_tensor("Yd", [NRX, 1024], F32, kind="Internal"); b_Yd = c.buf("Yd")
    fvd = nc.dram_tensor("fvd", [20, FVR, FVL], BF16, kind="Internal")
    vnd = nc.dram_tensor("vnd", [NS, 384], BF16, kind="Internal")
    b_fvd = c.buf("fvd"); b_vnd = c.buf("vnd")

    bcreg = nc.gpsimd.alloc_register("bcreg")
    nc.gpsimd.reg_mov(bcreg, NRX - 1)
    PS = nc.alloc_psum_tensor("ps", [128, 8, 512], F32)
    pb = c.bufs(8, "psb")

    def MM(out, lhsT, rhs, start, stop, rd, wr):
        c.op("pe", lambda e: e.matmul(out, lhsT=lhsT, rhs=rhs, start=start, stop=stop),
             reads=rd, writes=[wr], inc=stop)

    evc = [0]

    def EV(out, in_, rd, wr, scale=None, eng=None):
        if eng is None:
            eng = ("act", "dve")[evc[0] % 2]
            evc[0] += 1
        if eng == "act":
            c.op("act", lambda e: e.activation(out=out, in_=in_, func=AF.Copy,
                                               scale=(1.0 if scale is None else scale)),
                 reads=rd, writes=wr)
        else:
            if scale is None:
                c.op(eng, lambda e: e.tensor_copy(out=out, in_=in_), reads=rd, writes=wr)
            else:
                c.op(eng, lambda e: e.tensor_scalar(out=out, in0=in_, scalar1=float(scale), scalar2=None,
                                                    op0=ALU.mult), reads=rd, writes=wr)

    def AP(t, off, dims):
        return bass.AP(tensor=t, offset=off, ap=[list(d) for d in dims])

    es = ExitStack()
    es2 = ExitStack()

    def sb(name, shape, dt, stack=None):
        return (stack or es).enter_context(nc.sbuf_tensor(name, list(shape), dt))

    ident_b = sb("ident_b", [128, 128], BF16); b_ident = c.buf("ident")
    ident_f = sb("ident_f", [128, 128], F32); b_identf = c.buf("identf")
    ones_b = sb("ones_b", [128, 128], BF16); b_ones = c.buf("ones")
    c.dma("pool", lambda q: q.dma_start(out=ident_b[:, :], in_=ident), writes=[b_ident])
    c.dma("sp", lambda q: q.dma_start(out=ident_f[:, :], in_=ident), writes=[b_identf])
    c.op("dve", lambda e: e.memset(ones_b[:, :], 1.0), writes=[b_ones])
    hm = sb("hm", [128, 1], F32); b_hm = c.buf("hm")
    gates4 = sb("gates4", [128, 17, 4], F32); b_gates4 = c.buf("gates4")
    dest4 = sb("dest4", [128, 17, 4], I32); b_dest4 = c.buf("dest4")
    uT = sb("uT", [128, 8, NTOK], BF16); b_uT = c.buf("uT")
    c.op("dve", lambda e: e.memset(dest4[:, :, :], 1 << 30), writes=[b_dest4])
    c.dma("sp", lambda q: q.dma_start(out=hm[:, :], in_=hmask), writes=[b_hm])

    rbx = sb("rbx", [33, 20], BF16, es2); b_rbx = c.buf("rbx")
    c.dma("pool", lambda q: q.dma_start(out=rbx[0:32, :], in_=rel_bias), writes=[b_rbx])
    c.op("dve", lambda e: e.memset(rbx[32:33, :], 1.0), reads=[b_rbx], writes=[b_rbx])
    ohs = sb("ohs", [33, 4, FVL], BF16, es2); b_ohs = c.buf("ohs")
    c.dma("pool", lambda q: q.dma_start(out=ohs[:, :, :], in_=oh), writes=[b_ohs])
    fvs = sb("fvs", [8, 4, FVL], BF16, es2); b_fvs = c.buf("fvs")
    hsets = [(0, 8), (8, 4), (12, 4), (16, 4)]
    for s, (h0, nh) in enumerate(hsets):
        MM(PS[0:nh, s, 0:FVL], rbx[:, h0:h0 + nh], ohs[:, s, :], True, True, [b_rbx, b_ohs], pb[s])
        EV(fvs[0:nh, s, :], PS[0:nh, s, 0:FVL], [pb[s]], [b_fvs])
    for s, (h0, nh) in enumerate(hsets):
        src = AP(fvs.tensor if hasattr(fvs, "tensor") else fvs, s * FVL, [[4 * FVL, nh], [0, FVR], [1, FVL]])
        c.dma("sp", lambda q: q.dma_start(out=fvd.ap()[h0:h0 + nh, :, :], in_=src),
              reads=[b_fvs], writes=[b_fvd], own=b_fvd)
    biasA = sb("biasA", [128, 3, 8, 128], BF16, es2); b_biasA = c.buf("biasA")
    biasB = sb("biasB", [128, 3, 3, 4, 128], BF16, es2); b_biasB = c.buf("biasB")

    def toep(dst, h0, nh, cc, wb):
        src = AP(fvd, h0 * FVR * FVL + cc, [[FVL - 1, 128], [FVR * FVL, nh], [1, 128]])
        c.dma("sp", lambda q: q.dma_start(out=dst, in_=src), reads=[b_fvd], writes=[wb], own=wb)

    for ty, cc in ((0, 255), (1, 127)):
        toep(biasA[:, ty, :, :], 0, 8, cc, b_biasA)
        for g in range(3):
            toep(biasB[:, g, ty, :, :], 8 + 4 * g, 4, cc, b_biasB)
    c.op("dve", lambda e: e.tensor_scalar(out=biasA[:, 2, :, :], in0=biasA[:, 0, :, :], scalar1=hm[:, 0:1],
                                          scalar2=None, op0=ALU.add), reads=[b_biasA, b_hm], writes=[b_biasA])
    for g in range(3):
        c.op("dve", lambda e: e.tensor_scalar(out=biasB[:, g, 2, :, :], in0=biasB[:, g, 0, :, :],
                                              scalar1=hm[:, 0:1], scalar2=None, op0=ALU.add),
             reads=[b_biasB, b_hm], writes=[b_biasB])

    def load_w(dst, src2d, c0, c1, wbuf, nk=8):
        v = src2d.rearrange("(k p) n -> p k n", p=128)
        c.dma("pool", lambda q: q.dma_start(out=dst, in_=v[:, :, c0:c1]), writes=[wbuf], own=wbuf)

    obT = sb("obT", [64, 4, NTOK], BF16, es2); b_obT = c.buf("obT")

    xsT = sb("xsT", [128, 8, NS], BF16, es2); b_xsT = c.buf("xsT")
    c.dma("pool", lambda q: q.dma_start(out=xsT[:, :, :], in_=xst.rearrange("(k p) n -> p k n", p=128)),
          writes=[b_xsT])

    with ExitStack() as ph:
        wB = sb("wB", [128, 8, 1280], BF16, ph); b_wB = c.buf("wB")
        load_w(wB[:, :, :], w_in, 768, 2048, b_wB)
        kvo = [sb("kvoB%d" % i, [128, 512], F32, ph) for i in range(2)]
        b_kvo = c.bufs(2, "kvoB")
        qsT = sb("qsTB", [64, 12, NS], BF16, ph); b_qsT = c.buf("qsTB")
        ksT = sb("ksTB", [64, 4, NS], BF16, ph); b_ksT = c.buf("ksTB")
        ph1 = ExitStack()
        xp = [sb("xpB%d" % i, [128, 8, 512], BF16, ph1) for i in range(2)]
        b_xp = c.bufs(2, "xpB")
        pT = [sb("pTB%d" % i, [128, 512], BF16, ph1) for i in range(2)]
        b_pT = c.bufs(2, "pTB")
        KT = sb("KTB", [64, 2, 32 * 128], BF16, ph1); b_KT = c.buf("KTB")
        VV = sb("VVB", [128, 32, 128], BF16, ph1); b_VV = c.buf("VVB")
        QT = sb("QTB", [64, 2, NT], BF16, ph1); b_QT = c.buf("QTB")
        b_KT1 = c.buf("KTB1"); b_QT1 = c.buf("QTB1")
        stgK = [sb("stgK%d" % i, [128, 512], BF16, ph1) for i in range(2)]; b_stgK = c.bufs(2, "stgK")
        stgQ = [sb("stgQ%d" % i, [128, 512], BF16, ph1) for i in range(2)]; b_stgQ = c.bufs(2, "stgQ")
        acc = sb("accB", [64, 4, NT], F32, ph1); b_acc = c.buf("accB")
        npiece = 0
        for hp in range(2):
            for g in range(3):
                nH = NH_G[g]
                nblk = nH + 16
                xv = xt[g].rearrange("(k p) n -> p k n", p=128)
                for p0 in range(0, nblk, 4):
                    nb_ = min(4, nblk - p0)
                    ntk = nb_ * 128
                    xb = npiece % 2
                    npiece += 1
                    c.dma("pool", lambda q: q.dma_start(out=xp[xb][:, :, 0:ntk], in_=xv[:, :, p0 * 128:p0 * 128 + ntk]),
                          writes=[b_xp[xb]])
                    sgi = npiece % 2
                    for k in range(8):
                        MM(PS[:, 0, 0:ntk], wB[:, k, 768 + 128 * hp:768 + 128 * hp + 128], xp[xb][:, k, 0:ntk],
                           k == 0, k == 7, [b_wB, b_xp[xb]], pb[0])
                    EV(KT[:, 0, p0 * 128:p0 * 128 + ntk], PS[0:64, 0, 0:ntk], [pb[0]], [b_KT])
                    EV(stgK[sgi][64:128, 0:ntk], PS[64:128, 0, 0:ntk], [pb[0]], [b_stgK[sgi]])
                    c.dma("sp", lambda q: q.dma_start(out=KT[:, 1, p0 * 128:p0 * 128 + ntk], in_=stgK[sgi][64:128, 0:ntk]),
                          reads=[b_stgK[sgi]], writes=[b_KT1], own=b_stgK[sgi])
                    o0 = max(p0, nH)
                    if o0 < p0 + nb_:
                        lo = (o0 - p0) * 128
                        nq = ntk - lo
                        qc = slice((o0 - nH) * 128, (o0 - nH) * 128 + nq)
                        for k in range(8):
                            MM(PS[:, 1, 0:nq], wB[:, k, 256 * g + 128 * hp:256 * g + 128 * hp + 128],
                               xp[xb][:, k, lo:ntk], k == 0, k == 7, [b_wB, b_xp[xb]], pb[1])
                        EV(QT[:, 0, qc], PS[0:64, 1, 0:nq], [pb[1]], [b_QT], scale=0.125)
                        EV(stgQ[sgi][64:128, 0:nq], PS[64:128, 1, 0:nq], [pb[1]], [b_stgQ[sgi]], scale=0.125)
                        c.dma("sp", lambda q: q.dma_start(out=QT[:, 1, qc], in_=stgQ[sgi][64:128, 0:nq]),
                              reads=[b_stgQ[sgi]], writes=[b_QT1], own=b_stgQ[sgi])
                    for j in range(nb_):
                        blk = p0 + j
                        for k in range(8):
                            MM(PS[:, 4, j * 128:j * 128 + 128], xp[xb][:, k, j * 128:j * 128 + 128],
                               wB[:, k, 1024 + 128 * hp:1024 + 128 * hp + 128], k == 0, k == 7,
                               [b_wB, b_xp[xb]], pb[4])
                    EV(VV[:, p0:p0 + nb_, :], PS[:, 4, 0:ntk].rearrange("p (j n) -> p j n", n=128), [pb[4]], [b_VV])
                    if g == 0 and hp == 0:
                        for j in range(nb_):
                            blk = p0 + j
                            if blk < nH:
                                continue
                            ko = blk % 2
                            for k in range(8):
                                MM(PS[:, 5, :], xp[xb][:, k, j * 128:j * 128 + 128], wB[:, k, 768:1280],
                                   k == 0, k == 7, [b_wB, b_xp[xb]], pb[5])
                            EV(kvo[ko][:, :], PS[:, 5, :], [pb[5]], [b_kvo[ko]])
                            t0 = (blk - nH) * 128
                            c.dma("sp", lambda q: q.dma_start(out=bk_o[t0:t0 + 128, :], in_=kvo[ko][:, 0:256]),
                                  reads=[b_kvo[ko]], own=b_kvo[ko], final=True)
                            c.dma("sp", lambda q: q.dma_start(out=bv_o[t0:t0 + 128, :], in_=kvo[ko][:, 256:512]),
                                  reads=[b_kvo[ko]], own=b_kvo[ko], final=True)
                for j in range(16):
                    cur = nH + j
                    if g == 0:
                        prev, halo = cur - 1, (j == 0)
                    elif g == 1:
                        r, m = j // 4, j % 4
                        prev, halo = (r, True) if m == 0 else (cur - 1, False)
                    else:
                        prev, halo = j, True
                    sbk_ = 6 + (j % 2)
                    pi = j % 2
                    for ty, kb_ in ((0, prev), (1, cur)):
                        bty = 2 if (ty == 0 and halo) else ty
                        for hh in range(2):
                            MM(PS[:, sbk_, ty * 256 + hh * 128:ty * 256 + hh * 128 + 128],
                               KT[:, hh, kb_ * 128:kb_ * 128 + 128], QT[:, hh, j * 128:j * 128 + 128],
                               True, False, [b_KT1, b_QT1] if hh else [b_KT, b_QT], pb[sbk_])
                        MM(PS[:, sbk_, ty * 256:ty * 256 + 256], ident_b[:, :],
                           biasB[:, g, bty, 2 * hp:2 * hp + 2, :], False, True, [b_ident, b_biasB], pb[sbk_])
                    c.op("act", lambda e: e.activation(out=pT[pi][:, :], in_=PS[:, sbk_, :], func=AF.Exp),
                         reads=[pb[sbk_]], writes=[b_pT[pi]])
                    ob_ = 2 + (j % 2)
                    for hh in range(2):
                        MM(PS[0:64, ob_, hh * 128:hh * 128 + 128], VV[:, prev, hh * 64:hh * 64 + 64],
                           pT[pi][:, hh * 128:hh * 128 + 128], True, False, [b_VV, b_pT[pi]], pb[ob_])
                        MM(PS[0:64, ob_, hh * 128:hh * 128 + 128], VV[:, cur, hh * 64:hh * 64 + 64],
                           pT[pi][:, 256 + hh * 128:256 + hh * 128 + 128], False, True, [b_VV, b_pT[pi]], pb[ob_])
                    MM(PS[0:64, ob_, 256:512], ones_b[:, 0:64], pT[pi][:, 0:256], True, False, [b_ones, b_pT[pi]], pb[ob_])
                    MM(PS[0:64, ob_, 256:512], ones_b[:, 0:64], pT[pi][:, 256:512], False, True, [b_ones, b_pT[pi]], pb[ob_])
                    if g == 0:
                        off, st = j * 128, 1
                    elif g == 1:
                        off, st = 512 * (j % 4) + (j // 4), 4
                    else:
                        off, st = j, 16
                    av_ = AP(acc, off, [[4 * NT, 64], [NT, 4], [st, 128]])
                    src = PS[0:64, ob_, :].rearrange("p (a n) -> p a n", n=128)
                    if g == 0:
                        c.op("dve", lambda e: e.tensor_copy(out=av_, in_=src), reads=[pb[ob_]], writes=[b_acc])
                    else:
                        c.op("dve", lambda e: e.tensor_tensor(out=av_, in0=src, in1=av_, op=ALU.add),
                             reads=[pb[ob_], b_acc], writes=[b_acc])
            c.op("dve", lambda e: e.reciprocal(out=acc[:, 2:4, :], in_=acc[:, 2:4, :]), reads=[b_acc], writes=[b_acc])
            c.op("dve", lambda e: e.tensor_tensor(out=obT[:, 2 * hp:2 * hp + 2, 0:NT], in0=acc[:, 0:2, :],
                                                  in1=acc[:, 2:4, :], op=ALU.mult),
                 reads=[b_acc], writes=[b_obT])
        c.barrier()
        ph1.close()
        for hd in range(16):
            col = (64 * hd) if hd < 12 else (768 + 64 * (hd - 12))
            bk = hd % 2
            for k in range(8):
                MM(PS[0:64, bk, 0:NS], wB[:, k, col:col + 64], xsT[:, k, :], k == 0, k == 7, [b_wB, b_xsT], pb[bk])
            if hd < 12:
                EV(qsT[:, hd, :], PS[0:64, bk, 0:NS], [pb[bk]], [b_qsT], scale=0.125)
            else:
                EV(ksT[:, hd - 12, :], PS[0:64, bk, 0:NS], [pb[bk]], [b_ksT])
        for k in range(8):
            MM(PS[0:NS, 5, :], xsT[:, k, :], wB[:, k, 768:1280], k == 0, k == 7, [b_wB, b_xsT], pb[5])
        EV(kvo[0][0:NS, :], PS[0:NS, 5, :], [pb[5]], [b_kvo[0]])
        c.dma("sp", lambda q: q.dma_start(out=sbk_o[:, :], in_=kvo[0][0:NS, 0:256]), reads=[b_kvo[0]], own=b_kvo[0], final=True)
        c.dma("sp", lambda q: q.dma_start(out=sbv_o[:, :], in_=kvo[0][0:NS, 256:512]), reads=[b_kvo[0]], own=b_kvo[0], final=True)
        vtmp = sb("vtmpB", [NS, 256], BF16, ph); b_vtmp = c.buf("vtmpB")
        EV(vtmp[:, :], kvo[0][0:NS, 256:512], [b_kvo[0]], [b_vtmp], eng="dve")
        c.dma("sp", lambda q: q.dma_start(out=vnd.ap()[:, 128:384], in_=vtmp[:, :]), reads=[b_vtmp], writes=[b_vnd], own=b_vtmp)
        vnB = sb("vnB", [4, NB, 256], BF16, ph); b_vnB = c.buf("vnB")
        c.dma("sp", lambda q: q.dma_start(out=vnB[:, :, :], in_=vnd.ap()[:, 128:384].rearrange("(b i) n -> i b n", i=4)),
              reads=[b_vnd], writes=[b_vnB])
        bbc = sb("bbc", [128, 3, 4, 4], F32, ph); b_bbc = c.buf("bbc")
        bbn = sb("bbn", [4, 3, 4, 4], F32, ph); b_bbn = c.buf("bbn")
        offd = sb("offd", [4, 4], F32, ph); b_offd = c.buf("offd")
        c.op("dve", lambda e: e.tensor_scalar(out=offd[:, :], in0=ident_f[0:4, 0:4], scalar1=-1.0, scalar2=-NEG,
                                              op0=ALU.add, op1=ALU.mult), reads=[b_identf], writes=[b_offd])
        c.op("dve", lambda e: e.tensor_copy(out=bbc[:, 0, :, :], in_=biasB[:, 0, 0, :, 0:4]), reads=[b_biasB], writes=[b_bbc])
        c.op("dve", lambda e: e.tensor_copy(out=bbn[:, 0, :, :], in_=biasB[0:4, 0, 1, :, 0:4]), reads=[b_biasB], writes=[b_bbn])
        for g in (1, 2):
            c.op("dve", lambda e: e.tensor_copy(out=bbc[:, g, :, :], in_=biasB[:, g, 0, :, 0:1].to_broadcast([128, 4, 4])),
                 reads=[b_biasB], writes=[b_bbc])
            c.op("dve", lambda e: e.tensor_tensor(out=bbn[:, g, :, :], in0=biasB[0:4, g, 1, :, 0:4],
                                                  in1=offd[:, :].unsqueeze(1).to_broadcast([4, 4, 4]), op=ALU.add),
                 reads=[b_biasB, b_offd], writes=[b_bbn])
        kcb = [sb("kcb%d" % i, [64, 9, 4, 128], BF16, ph) for i in range(2)]; b_kcb = c.bufs(2, "kcb")
        vcb = [sb("vcb%d" % i, [128, 9, 4, 64], BF16, ph) for i in range(2)]; b_vcb = c.bufs(2, "vcb")
        sc_f = sb("scfB", [128, 48], F32, ph); b_scf = c.buf("scfB")
        sn_f = sb("snfB", [4, 48], F32, ph); b_snf = c.buf("snfB")
        pc_b = sb("pcB", [128, 48], BF16, ph); b_pc = c.buf("pcB")
        pn_b = sb("pnB", [4, 48], BF16, ph); b_pn = c.buf("pnB")
        pns = sb("pnsB", [4, 16], BF16, ph); b_pns = c.buf("pnsB")
        rcs = sb("rcsB", [64, 16], F32, ph); b_rcs = c.buf("rcsB")
        for b in range(NB):
            bb = b % 2
            c.dma("pool", lambda q: q.dma_start(out=kcb[bb][:, :, :, :].rearrange("p s h n -> p (s h n)"), in_=cbkT[b]),
                  writes=[b_kcb[bb]])
            c.dma("pool", lambda q: q.dma_start(out=vcb[bb][:, :, :, :].rearrange("p s h n -> p (s h n)"), in_=cbv[b]),
                  writes=[b_vcb[bb]])
            tk = slice(4 * b, 4 * b + 4)
            for h in range(4):
                MM(PS[:, 0, h * 4:h * 4 + 4], kcb[bb][:, 0, h, :], qsT[:, h, tk], True, True, [b_kcb[bb], b_qsT], pb[0])
                for g in (1, 2):
                    for i in range(4):
                        cix = g * 16 + h * 4 + i
                        MM(PS[:, 0, cix:cix + 1], kcb[bb][:, 1 + 4 * (g - 1) + i, h, :],
                           qsT[:, 4 * g + h, 4 * b + i:4 * b + i + 1], True, True, [b_kcb[bb], b_qsT], pb[0])
                MM(PS[0:4, 1, :48].rearrange("p (g h i) -> p g h i", g=3, h=4)[:, :, h, :], ksT[:, h, tk],
                   AP(qsT, h * NS + 4 * b, [[12 * NS, 64], [4 * NS, 3], [1, 4]]), True, True, [b_ksT, b_qsT], pb[1])
            c.op("dve", lambda e: e.tensor_tensor(out=sc_f[:, :], in0=PS[:, 0, 0:48],
                                                  in1=bbc[:, :, :, :].rearrange("p g h i -> p (g h i)"), op=ALU.add),
                 reads=[pb[0], b_bbc], writes=[b_scf])
            c.op("dve", lambda e: e.tensor_tensor(out=sn_f[:, :], in0=PS[0:4, 1, 0:48],
                                                  in1=bbn[:, :, :, :].rearrange("p g h i -> p (g h i)"), op=ALU.add),
                 reads=[pb[1], b_bbn], writes=[b_snf])
            c.op("act", lambda e: e.activation(out=pc_b[:, :], in_=sc_f[:, :], func=AF.Exp), reads=[b_scf], writes=[b_pc])
            c.op("act", lambda e: e.activation(out=pn_b[:, :], in_=sn_f[:, :], func=AF.Exp), reads=[b_snf], writes=[b_pn])
            c.op("dve", lambda e: e.tensor_tensor(out=pns[:, :], in0=pn_b[:, 0:16], in1=pn_b[:, 16:32], op=ALU.add),
                 reads=[b_pn], writes=[b_pns])
            c.op("dve", lambda e: e.tensor_tensor(out=pns[:, :], in0=pns[:, :], in1=pn_b[:, 32:48], op=ALU.add),
                 reads=[b_pn, b_pns], writes=[b_pns])
            for h in range(4):
                MM(PS[0:64, 2, h * 4:h * 4 + 4], vcb[bb][:, 0, h, :], pc_b[:, h * 4:h * 4 + 4], True, False,
                   [b_vcb[bb], b_pc], pb[2])
                for g in (1, 2):
                    for i in range(4):
                        cix = g * 16 + h * 4 + i
                        MM(PS[0:64, 2, h * 4 + i:h * 4 + i + 1], vcb[bb][:, 1 + 4 * (g - 1) + i, h, :],
                           pc_b[:, cix:cix + 1], False, False, [b_vcb[bb], b_pc], pb[2])
                MM(PS[0:64, 2, h * 4:h * 4 + 4], vnB[:, b, 64 * h:64 * h + 64], pns[:, h * 4:h * 4 + 4], False, True,
                   [b_vnB, b_pns], pb[2])
            for g in range(3):
                MM(PS[0:64, 3, 0:16], ones_b[:, 0:64], pc_b[:, g * 16:g * 16 + 16], g == 0, False, [b_ones, b_pc], pb[3])
            MM(PS[0:64, 3, 0:16], ones_b[0:4, 0:64], pns[:, :], False, True, [b_ones, b_pns], pb[3])
            c.op("dve", lambda e: e.reciprocal(out=rcs[:, :], in_=PS[0:64, 3, 0:16]), reads=[pb[3]], writes=[b_rcs])
            c.op("dve", lambda e: e.tensor_tensor(out=obT[:, :, NT + 4 * b:NT + 4 * b + 4],
                                                  in0=PS[0:64, 2, 0:16].rearrange("p (h i) -> p h i", i=4),
                                                  in1=rcs[:, :].rearrange("p (h i) -> p h i", i=4), op=ALU.mult),
                 reads=[pb[2], b_rcs], writes=[b_obT])
        c.barrier()
        if stop == "B":
            c.finish("sp")
            return nc

    oaT = sb("oaT", [64, 8, NTOK], BF16, es2); b_oaT = c.buf("oaT")
    xT = sb("xT", [128, 8, 17 * 128], BF16, es2); b_xT = c.buf("xT")
    c.dma("pool", lambda q: q.dma_start(out=xT[:, :, :], in_=xt[0].rearrange("(k p) n -> p k n", p=128)), writes=[b_xT])
    es8 = sb("es8", [64, 8], F32, es2); b_es8 = c.buf("es8")
    c.dma("sp", lambda q: q.dma_start(out=es8[:, :], in_=AP(sinks.tensor, 0, [[0, 64], [1, 8]])), writes=[b_es8])
    c.op("act", lambda e: e.activation(out=es8[:, :], in_=es8[:, :], func=AF.Exp), reads=[b_es8], writes=[b_es8])
    with ExitStack() as ph:
        wA = sb("wA", [128, 8, 768], BF16, ph); b_wA = c.buf("wA")
        load_w(wA[:, :, :], w_in, 0, 768, b_wA)
        dtm = sb("dtmA", [64, 512], F32, ph); b_dtm = c.buf("dtmA")
        kvoA = sb("kvoA", [128, 256], F32, ph); b_kvoA = c.buf("kvoA")
        ph1 = ExitStack()
        KTA = sb("KTA", [64, 2, 17 * 128], BF16, ph1); b_KTA = c.buf("KTA")
        VA = sb("VA", [128, 17, 128], BF16, ph1); b_VA = c.buf("VA")
        QTA = sb("QTA", [64, 8, NT], BF16, ph1); b_QTA = c.buf("QTA")
        pTA = [sb("pTA%d" % i, [128, 2, 512], BF16, ph1) for i in range(2)]; b_pTA = c.bufs(2, "pTA")
        for p0 in range(0, 17, 4):
            nb_ = min(4, 17 - p0)
            ntk = nb_ * 128
            cs = slice(p0 * 128, p0 * 128 + ntk)
            for g in range(2):
                for k in range(8):
                    MM(PS[0:64, g, 0:ntk], wA[:, k, 512 + 64 * g:512 + 64 * g + 64], xT[:, k, cs], k == 0, k == 7,
                       [b_wA, b_xT], pb[g])
                EV(KTA[:, g, cs], PS[0:64, g, 0:ntk], [pb[g]], [b_KTA])
            o0 = max(p0, 1)
            lo = (o0 - p0) * 128
            nq = ntk - lo
            if nq > 0:
                for h in range(8):
                    bq = 2 + (h % 2)
                    for k in range(8):
                        MM(PS[0:64, bq, 0:nq], wA[:, k, 64 * h:64 * h + 64], xT[:, k, o0 * 128:o0 * 128 + nq], k == 0, k == 7,
                           [b_wA, b_xT], pb[bq])
                    EV(QTA[:, h, (o0 - 1) * 128:(o0 - 1) * 128 + nq], PS[0:64, bq, 0:nq], [pb[bq]], [b_QTA], scale=0.125)
            for j in range(nb_):
                for k in range(8):
                    MM(PS[:, 4, j * 128:j * 128 + 128], xT[:, k, (p0 + j) * 128:(p0 + j) * 128 + 128], wA[:, k, 640:768],
                       k == 0, k == 7, [b_wA, b_xT], pb[4])
            EV(VA[:, p0:p0 + nb_, :], PS[:, 4, 0:ntk].rearrange("p (j n) -> p j n", n=128), [pb[4]], [b_VA])
        for k in range(8):
            MM(PS[:, 5, 0:256], xT[:, k, 16 * 128:17 * 128], wA[:, k, 512:768], k == 0, k == 7, [b_wA, b_xT], pb[5])
        EV(kvoA[:, :], PS[:, 5, 0:256], [pb[5]], [b_kvoA])
        c.dma("sp", lambda q: q.dma_start(out=ak_o[:, :], in_=kvoA[:, 0:128]), reads=[b_kvoA], own=b_kvoA, final=True)
        c.dma("sp", lambda q: q.dma_start(out=av_o[:, :], in_=kvoA[:, 128:256]), reads=[b_kvoA], own=b_kvoA, final=True)
        for gg in range(2):
            for j in range(16):
                pi = j % 2
                for ty, kb_ in ((0, j), (1, j + 1)):
                    bty = 2 if (ty == 0 and j == 0) else ty
                    sbk_ = 6 + ty
                    MM(PS[:, sbk_, :], KTA[:, gg, kb_ * 128:kb_ * 128 + 128], QTA[:, 4 * gg:4 * gg + 4, j * 128:j * 128 + 128],
                       True, False, [b_KTA, b_QTA], pb[sbk_])
                    MM(PS[:, sbk_, :], ident_b[:, :], biasA[:, bty, 4 * gg:4 * gg + 4, :], False, True,
                       [b_ident, b_biasA], pb[sbk_])
                    c.op("act", lambda e: e.activation(out=pTA[pi][:, ty, :], in_=PS[:, sbk_, :], func=AF.Exp),
                         reads=[pb[sbk_]], writes=[b_pTA[pi]])
                ob_, db_ = 0 + (j % 2), 2 + (j % 2)
                MM(PS[0:64, ob_, :], VA[:, j, 64 * gg:64 * gg + 64], pTA[pi][:, 0, :], True, False, [b_VA, b_pTA[pi]], pb[ob_])
                MM(PS[0:64, ob_, :], VA[:, j + 1, 64 * gg:64 * gg + 64], pTA[pi][:, 1, :], False, True, [b_VA, b_pTA[pi]], pb[ob_])
                MM(PS[0:64, db_, :], ones_b[:, 0:64], pTA[pi][:, 0, :], True, False, [b_ones, b_pTA[pi]], pb[db_])
                MM(PS[0:64, db_, :], ones_b[:, 0:64], pTA[pi][:, 1, :], False, True, [b_ones, b_pTA[pi]], pb[db_])
                c.op("dve", lambda e: e.tensor_tensor(out=dtm[:, :].rearrange("p (h n) -> p h n", n=128),
                                                      in0=PS[0:64, db_, :].rearrange("p (h n) -> p h n", n=128),
                                                      in1=es8[:, 4 * gg:4 * gg + 4].unsqueeze(2).to_broadcast([64, 4, 128]),
                                                      op=ALU.add), reads=[pb[db_], b_es8], writes=[b_dtm])
                c.op("dve", lambda e: e.reciprocal(out=dtm[:, :], in_=dtm[:, :]), reads=[b_dtm], writes=[b_dtm])
                c.op("dve", lambda e: e.tensor_tensor(out=oaT[:, 4 * gg:4 * gg + 4, j * 128:j * 128 + 128],
                                                      in0=PS[0:64, ob_, :].rearrange("p (h n) -> p h n", n=128),
                                                      in1=dtm[:, :].rearrange("p (h n) -> p h n", n=128), op=ALU.mult),
                     reads=[pb[ob_], b_dtm], writes=[b_oaT])
        c.barrier()
        ph1.close()
        qaS = sb("qaS", [64, 8, NS], BF16, ph); b_qaS = c.buf("qaS")
        kaS = sb("kaS", [64, 2, NS], BF16, ph); b_kaS = c.buf("kaS")
        for hd in range(10):
            col = 64 * hd
            bk = hd % 2
            for k in range(8):
                MM(PS[0:64, bk, 0:NS], wA[:, k, col:col + 64], xsT[:, k, :], k == 0, k == 7, [b_wA, b_xsT], pb[bk])
            if hd < 8:
                EV(qaS[:, hd, :], PS[0:64, bk, 0:NS], [pb[bk]], [b_qaS], scale=0.125)
            else:
                EV(kaS[:, hd - 8, :], PS[0:64, bk, 0:NS], [pb[bk]], [b_kaS])
        for k in range(8):
            MM(PS[0:NS, 5, 0:256], xsT[:, k, :], wA[:, k, 512:768], k == 0, k == 7, [b_wA, b_xsT], pb[5])
        EV(kvoA[0:NS, :], PS[0:NS, 5, 0:256], [pb[5]], [b_kvoA])
        c.dma("sp", lambda q: q.dma_start(out=sak_o[:, :], in_=kvoA[0:NS, 0:128]), reads=[b_kvoA], own=b_kvoA, final=True)
        c.dma("sp", lambda q: q.dma_start(out=sav_o[:, :], in_=kvoA[0:NS, 128:256]), reads=[b_kvoA], own=b_kvoA, final=True)
        vtA = sb("vtA", [NS, 128], BF16, ph); b_vtA = c.buf("vtA")
        EV(vtA[:, :], kvoA[0:NS, 128:256], [b_kvoA], [b_vtA], eng="dve")
        c.dma("sp", lambda q: q.dma_start(out=vnd.ap()[:, 0:128], in_=vtA[:, :]), reads=[b_vtA], writes=[b_vnd], own=b_vtA)
        vnA = sb("vnA", [4, NB, 128], BF16, ph); b_vnA = c.buf("vnA")
        c.dma("sp", lambda q: q.dma_start(out=vnA[:, :, :], in_=vnd.ap()[:, 0:128].rearrange("(b i) n -> i b n", i=4)),
              reads=[b_vnd], writes=[b_vnA])
        KcA = sb("KcA", [64, NB, 2, 128], BF16, ph); b_KcA = c.buf("KcA")
        VcA = sb("VcA", [128, NB, 128], BF16, ph); b_VcA = c.buf("VcA")
        c.dma("pool", lambda q: q.dma_start(out=KcA[:, :, :, :], in_=cakT), writes=[b_KcA])
        c.dma("pool", lambda q: q.dma_start(out=VcA[:, :, :], in_=cav), writes=[b_VcA])
        scA = sb("scA", [128, 512], F32, ph); b_scA = c.buf("scA")
        snA = sb("snA", [4, 512], F32, ph); b_snA = c.buf("snA")
        pcA = sb("pcA", [128, 512], BF16, ph); b_pcA = c.buf("pcA")
        pnA = sb("pnA", [4, 512], BF16, ph); b_pnA = c.buf("pnA")
        for b in range(NB):
            for g in range(2):
                cs = slice((2 * b + g) * 16, (2 * b + g) * 16 + 16)
                qv = AP(qaS, 4 * g * NS + 4 * b, [[8 * NS, 64], [NS, 4], [1, 4]])
                MM(PS[:, 6, cs], KcA[:, b, g, :], qv, True, True, [b_KcA, b_qaS], pb[6])
                MM(PS[0:4, 7, cs], kaS[:, g, 4 * b:4 * b + 4], qv, True, True, [b_kaS, b_qaS], pb[7])
        c.op("dve", lambda e: e.tensor_tensor(out=scA[:, :].rearrange("p (b h i) -> p b h i", b=NB, h=8),
                                              in0=PS[:, 6, :].rearrange("p (b h i) -> p b h i", b=NB, h=8),
                                              in1=AP(biasA, 0, [[3072, 128], [0, NB], [128, 8], [1, 4]]), op=ALU.add),
             reads=[pb[6], b_biasA], writes=[b_scA])
        c.op("dve", lambda e: e.tensor_tensor(out=snA[:, :].rearrange("p (b h i) -> p b h i", b=NB, h=8),
                                              in0=PS[0:4, 7, :].rearrange("p (b h i) -> p b h i", b=NB, h=8),
                                              in1=AP(biasA, 1024, [[3072, 4], [0, NB], [128, 8], [1, 4]]), op=ALU.add),
             reads=[pb[7], b_biasA], writes=[b_snA])
        c.op("act", lambda e: e.activation(out=pcA[:, :], in_=scA[:, :], func=AF.Exp), reads=[b_scA], writes=[b_pcA])
        c.op("act", lambda e: e.activation(out=pnA[:, :], in_=snA[:, :], func=AF.Exp), reads=[b_snA], writes=[b_pnA])
        for b in range(NB):
            for g in range(2):
                cs = slice((2 * b + g) * 16, (2 * b + g) * 16 + 16)
                MM(PS[0:64, 0, cs], VcA[:, b, 64 * g:64 * g + 64], pcA[:, cs], True, False, [b_VcA, b_pcA], pb[0])
                MM(PS[0:64, 0, cs], vnA[:, b, 64 * g:64 * g + 64], pnA[:, cs], False, True, [b_vnA, b_pnA], pb[0])
        MM(PS[0:64, 1, :], ones_b[:, 0:64], pcA[:, :], True, False, [b_ones, b_pcA], pb[1])
        MM(PS[0:64, 1, :], ones_b[0:4, 0:64], pnA[:, :], False, True, [b_ones, b_pnA], pb[1])
        c.op("dve", lambda e: e.tensor_tensor(out=dtm[:, :].rearrange("p (b h i) -> p b h i", b=NB, h=8),
                                              in0=PS[0:64, 1, :].rearrange("p (b h i) -> p b h i", b=NB, h=8),
                                              in1=AP(es8, 0, [[8, 64], [0, NB], [1, 8], [0, 4]]), op=ALU.add),
             reads=[pb[1], b_es8], writes=[b_dtm])
        c.op("dve", lambda e: e.reciprocal(out=dtm[:, :], in_=dtm[:, :]), reads=[b_dtm], writes=[b_dtm])
        c.op("dve", lambda e: e.tensor_tensor(out=AP(oaT, NT, [[8 * NTOK, 64], [4, NB], [NTOK, 8], [1, 4]]),
                                              in0=PS[0:64, 0, :].rearrange("p (b h i) -> p b h i", b=NB, h=8),
                                              in1=dtm[:, :].rearrange("p (b h i) -> p b h i", b=NB, h=8), op=ALU.mult),
             reads=[pb[0], b_dtm], writes=[b_oaT])
        c.barrier()
        if stop == "A":
            c.finish("sp")
            return nc

    omT = sb("omT", [128, 4, NTOK], BF16, es2); b_omT = c.buf("omT")
    with ExitStack() as ph:
        wM = sb("wM", [128, 8, 512], BF16, ph); b_wM = c.buf("wM")
        load_w(wM[:, :, :], w_in, 2048, 2560, b_wM)
        wmk = sb("wmk", [128, 8, 1024], BF16, ph); b_wmk = c.buf("wmk")
        load_w(wmk[:, :, :], w_mem_kv, 0, 1024, b_wmk)
        mTs = sb("mTs", [128, 8, 256], BF16, ph); b_mTs = c.buf("mTs")
        c.dma("pool", lambda q: q.dma_start(out=mTs[:, :, :], in_=memT.rearrange("(k p) n -> p k n", p=128)), writes=[b_mTs])
        KMT = sb("KMT", [128, 4, 256], BF16, ph); b_KMT = c.buf("KMT")
        VM = sb("VM", [128, 2, 512], BF16, ph); b_VM = c.buf("VM")
        mo = [sb("moM0", [128, 512], F32, ph)] * 2; b_mo = [c.buf("moM")] * 2
        for h in range(4):
            for k in range(8):
                MM(PS[:, h % 2, 0:256], wmk[:, k, 128 * h:128 * h + 128], mTs[:, k, :], k == 0, k == 7, [b_wmk, b_mTs], pb[h % 2])
            EV(KMT[:, h, :], PS[:, h % 2, 0:256], [pb[h % 2]], [b_KMT])
        for t in range(2):
            for half, dst in ((0, mk_o), (1, mv_o)):
                bk = 2 + half
                for k in range(8):
                    MM(PS[:, bk, :], mTs[:, k, 128 * t:128 * t + 128], wmk[:, k, 512 * half:512 * half + 512], k == 0, k == 7,
                       [b_wmk, b_mTs], pb[bk])
                mi = (2 * t + half) % 2
                EV(mo[mi][:, :], PS[:, bk, :], [pb[bk]], [b_mo[mi]])
                if half == 1:
                    EV(VM[:, t, :], mo[mi][:, :], [b_mo[mi]], [b_VM], eng="dve")
                c.dma("sp", lambda q: q.dma_start(out=dst[128 * t:128 * t + 128, :], in_=mo[mi][:, :]),
                      reads=[b_mo[mi]], own=b_mo[mi], final=True)
        QMT = [sb("QMT%d" % i, [128, 512], BF16, ph) for i in range(2)]; b_QMT = c.bufs(2, "QMT")
        pM = [sb("pM%d" % i, [128, 2, 512], BF16, ph) for i in range(2)]; b_pM = c.bufs(2, "pM")
        dtM = sb("dtM", [128, 512], F32, ph); b_dtM = c.buf("dtM")
        MSC = float(128.0 ** -0.5)
        it = 0
        for h in range(4):
            for tg in range(4):
                qi = it % 2
                it += 1
                cs = slice(128 + 512 * tg, 128 + 512 * tg + 512)
                for k in range(8):
                    MM(PS[:, qi, :], wM[:, k, 128 * h:128 * h + 128], xT[:, k, cs], k == 0, k == 7, [b_wM, b_xT], pb[qi])
                EV(QMT[qi][:, :], PS[:, qi, :], [pb[qi]], [b_QMT[qi]], scale=MSC)
                for t in range(2):
                    MM(PS[:, 4 + t, :], KMT[:, h, 128 * t:128 * t + 128], QMT[qi][:, :], True, True, [b_KMT, b_QMT[qi]], pb[4 + t])
                    c.op("act", lambda e: e.activation(out=pM[qi][:, t, :], in_=PS[:, 4 + t, :], func=AF.Exp),
                         reads=[pb[4 + t]], writes=[b_pM[qi]])
                for t in range(2):
                    MM(PS[:, 6, :], VM[:, t, 128 * h:128 * h + 128], pM[qi][:, t, :], t == 0, t == 1, [b_VM, b_pM[qi]], pb[6])
                for t in range(2):
                    MM(PS[:, 7, :], ones_b[:, :], pM[qi][:, t, :], t == 0, t == 1, [b_ones, b_pM[qi]], pb[7])
                c.op("dve", lambda e: e.reciprocal(out=dtM[:, :], in_=PS[:, 7, :]), reads=[pb[7]], writes=[b_dtM])
                c.op("dve", lambda e: e.tensor_tensor(out=omT[:, h, 512 * tg:512 * tg + 512], in0=PS[:, 6, :], in1=dtM[:, :],
                                                      op=ALU.mult), reads=[pb[6], b_dtM], writes=[b_omT])
        qmS = sb("qmS", [128, 4, NS], BF16, ph); b_qmS = c.buf("qmS")
        for h in range(4):
            for k in range(8):
                MM(PS[:, h % 2, 0:NS], wM[:, k, 128 * h:128 * h + 128], xsT[:, k, :], k == 0, k == 7, [b_wM, b_xsT], pb[h % 2])
            EV(qmS[:, h, :], PS[:, h % 2, 0:NS], [pb[h % 2]], [b_qmS], scale=MSC)
        KcM = [sb("KcM%d" % i, [128, 4, 256], BF16, ph) for i in range(2)]; b_KcM = c.bufs(2, "KcM")
        VcM = [sb("VcM%d" % i, [128, 2, 512], BF16, ph) for i in range(2)]; b_VcM = c.bufs(2, "VcM")
        p32 = [sb("p32_%d" % i, [128, 32], BF16, ph) for i in range(2)]; b_p32 = c.bufs(2, "p32")
        for b in range(NB):
            bb = b % 2
            c.dma("pool", lambda q: q.dma_start(out=KcM[bb][:, :, :].rearrange("p h n -> p (h n)"), in_=cmkT[b]), writes=[b_KcM[bb]])
            c.dma("pool", lambda q: q.dma_start(out=VcM[bb][:, :, :].rearrange("p t n -> p (t n)"), in_=cmv[b]), writes=[b_VcM[bb]])
            sbk_ = 4 + bb
            for t in range(2):
                for h in range(4):
                    cix = t * 16 + h * 4
                    MM(PS[:, sbk_, cix:cix + 4], KcM[bb][:, h, 128 * t:128 * t + 128], qmS[:, h, 4 * b:4 * b + 4], True, True,
                       [b_KcM[bb], b_qmS], pb[sbk_])
            c.op("act", lambda e: e.activation(out=p32[bb][:, :], in_=PS[:, sbk_, 0:32], func=AF.Exp), reads=[pb[sbk_]], writes=[b_p32[bb]])
            for h in range(4):
                for t in range(2):
                    MM(PS[:, 6, b * 16 + h * 4:b * 16 + h * 4 + 4], VcM[bb][:, t, 128 * h:128 * h + 128],
                       p32[bb][:, t * 16 + h * 4:t * 16 + h * 4 + 4], t == 0, t == 1, [b_VcM[bb], b_p32[bb]], pb[6])
            for t in range(2):
                MM(PS[:, 7, b * 16:b * 16 + 16], ones_b[:, :], p32[bb][:, t * 16:t * 16 + 16], t == 0, t == 1, [b_ones, b_p32[bb]], pb[7])
        c.op("dve", lambda e: e.reciprocal(out=dtM[:, 0:256], in_=PS[:, 7, 0:256]), reads=[pb[7]], writes=[b_dtM])
        c.op("dve", lambda e: e.tensor_tensor(out=AP(omT, NT, [[4 * NTOK, 128], [4, NB], [NTOK, 4], [1, 4]]),
                                              in0=PS[:, 6, 0:256].rearrange("p (b h i) -> p b h i", b=NB, h=4),
                                              in1=dtM[:, 0:256].rearrange("p (b h i) -> p b h i", b=NB, h=4), op=ALU.mult),
             reads=[pb[6], b_dtM], writes=[b_omT])
        c.barrier()
        if stop == "M":
            c.finish("sp")
            return nc

    hT_d = nc.dram_tensor("hT_d", [128, 8, NTOK], BF16, kind="Internal"); b_hTd = c.buf("hTd")
    fa_d = nc.dram_tensor("fa_d", [17, 128, 1024], F32, kind="Internal"); b_fad = c.buf("fad")
    with ExitStack() as ph:
        wG = [sb("wG%d" % i, [128, 8, 3, 128], BF16, ph) for i in range(2)]; b_wG = c.bufs(2, "wG")
        wbA = [sb("wbA%d" % i, [64, 8, 128], BF16, ph) for i in range(2)]; b_wbA = c.bufs(2, "wbA")
        wbB = [sb("wbB%d" % i, [64, 4, 128], BF16, ph) for i in range(2)]; b_wbB = c.bufs(2, "wbB")
        wbM = [sb("wbM%d" % i, [128, 4, 128], BF16, ph) for i in range(2)]; b_wbM = c.bufs(2, "wbM")
        sg = [sb("sg%d" % i, [128, 512], F32, ph) for i in range(3)]; b_sg = c.bufs(3, "sg")
        wv = w_in.rearrange("(k p) n -> p k n", p=128)
        for cc in range(8):
            wi = cc % 2
            for br in range(3):
                c0 = 2560 + 1024 * br + 128 * cc
                c.dma("pool", lambda q: q.dma_start(out=wG[wi][:, :, br, :], in_=wv[:, :, c0:c0 + 128]),
                      writes=[b_wG[wi]], own=b_wG[wi])
            c.dma("pool", lambda q: q.dma_start(out=wbA[wi][:, :, :], in_=w_br_a.rearrange("(h d) n -> d h n", d=64)[:, :, 128 * cc:128 * cc + 128]),
                  writes=[b_wbA[wi]])
            c.dma("pool", lambda q: q.dma_start(out=wbB[wi][:, :, :], in_=w_br_b.rearrange("(h d) n -> d h n", d=64)[:, :, 128 * cc:128 * cc + 128]),
                  writes=[b_wbB[wi]])
            c.dma("pool", lambda q: q.dma_start(out=wbM[wi][:, :, :], in_=w_br_m.rearrange("(h d) n -> d h n", d=128)[:, :, 128 * cc:128 * cc + 128]),
                  writes=[b_wbM[wi]])
            for tg in range(5):
                ntk = 512 if tg < 4 else NS
                tc_ = slice(512 * tg, 512 * tg + ntk)
                for br in range(3):
                    for k in range(8):
                        xr = xT[:, k, 128 + 512 * tg:128 + 512 * tg + 512] if tg < 4 else xsT[:, k, :]
                        MM(PS[:, br, 0:ntk], wG[wi][:, k, br, :], xr, k == 0, k == 7, [b_wG[wi], b_xT, b_xsT], pb[br])
                    c.op("act", lambda e: e.activation(out=sg[br][:, 0:ntk], in_=PS[:, br, 0:ntk], func=AF.Sigmoid),
                         reads=[pb[br]], writes=[b_sg[br]])
                for h in range(8):
                    MM(PS[:, 3, 0:ntk], wbA[wi][:, h, :], oaT[:, h, tc_], h == 0, h == 7, [b_wbA[wi], b_oaT], pb[3])
                for h in range(4):
                    MM(PS[:, 4, 0:ntk], wbB[wi][:, h, :], obT[:, h, tc_], h == 0, h == 3, [b_wbB[wi], b_obT], pb[4])
                for h in range(4):
                    MM(PS[:, 5, 0:ntk], wbM[wi][:, h, :], omT[:, h, tc_], h == 0, h == 3, [b_wbM[wi], b_omT], pb[5])
                for br in range(3):
                    c.op("dve", lambda e: e.tensor_tensor(out=sg[br][:, 0:ntk], in0=sg[br][:, 0:ntk], in1=PS[:, 3 + br, 0:ntk],
                                                          op=ALU.mult), reads=[pb[3 + br], b_sg[br]], writes=[b_sg[br]])
                c.op("dve", lambda e: e.tensor_tensor(out=sg[0][:, 0:ntk], in0=sg[0][:, 0:ntk], in1=sg[1][:, 0:ntk], op=ALU.add),
                     reads=[b_sg[0], b_sg[1]], writes=[b_sg[0]])
                c.op("dve", lambda e: e.tensor_tensor(out=uT[:, cc, tc_], in0=sg[0][:, 0:ntk], in1=sg[2][:, 0:ntk], op=ALU.add),
                     reads=[b_sg[0], b_sg[2]], writes=[b_uT])
        c.barrier()
        if stop == "MERGE":
            c.finish("sp")
            return nc
    c.barrier()
    es2.close()
    NR = 10
    wp = [sb("wp%d" % i, [128, 8, 512], BF16) for i in range(NR)]; b_wp = c.bufs(NR, "wp")
    bdn = [sb("bdn%d" % i, [1, 1024], BF16) for i in range(4)]; b_bdn = c.bufs(4, "bdn")
    piece_slot = {}
    npiece_issued = [0]

    def issue_piece():
        n = npiece_issued[0]
        npiece_issued[0] += 1
        e_, p = n // 6, n % 6
        sl = n % NR
        piece_slot[n] = sl
        if p == 0:
            c.dma("pool", lambda q: q.dma_start(out=bdn[e_ % 4][:, :], in_=b_down[e_:e_ + 1, :]), writes=[b_bdn[e_ % 4]])
        src = w_gu[e_, p] if p < 4 else w_down[e_, p - 4]
        c.dma("pool", lambda q: q.dma_start(out=wp[sl][:, :, :].rearrange("p k n -> p (k n)"), in_=src),
              writes=[b_wp[sl]], own=b_wp[sl])

    for _ in range(min(NR, 6 * n_experts)):
        issue_piece()
    with ExitStack() as ph:
        wo = sb("wo", [128, 8, 1024], BF16, ph); b_wo = c.buf("wo")
        load_w(wo[:, :, :], w_o, 0, 1024, b_wo)
        wr = sb("wr", [128, 8, 32], F32, ph); b_wr = c.buf("wr")
        c.dma("sp", lambda q: q.dma_start(out=wr[:, :, :], in_=w_router.rearrange("(k p) n -> p k n", p=128)), writes=[b_wr])
        lnp = sb("lnp", [128, 2, 1024], F32, ph); brt = sb("brt", [128, 32], F32, ph); b_lnp = c.buf("lnp")
        c.dma("sp", lambda q: q.dma_start(out=lnp[:, 0, :], in_=AP(ln1_g.tensor, 0, [[0, 128], [1, 1024]])), writes=[b_lnp], own=b_lnp)
        c.dma("sp", lambda q: q.dma_start(out=lnp[:, 1, :], in_=AP(ln1_b.tensor, 0, [[0, 128], [1, 1024]])), writes=[b_lnp], own=b_lnp)
        c.dma("sp", lambda q: q.dma_start(out=brt[:, :], in_=AP(b_router.tensor, 0, [[0, 128], [1, 32]])), writes=[b_lnp], own=b_lnp)
        xk = [sb("xk%d" % i, [128, 1024], F32, ph) for i in range(2)]; b_xk = c.bufs(2, "xk")
        zz = [sb("zz%d" % i, [128, 1024], F32, ph) for i in range(2)]; b_zz = c.bufs(2, "zz")
        fa = [sb("fa%d" % i, [128, 1024], F32, ph) for i in range(2)]; b_fa = c.bufs(2, "fa")
        hbs = [sb("hb%d" % i, [128, 1024], BF16, ph) for i in range(4)]; b_hbs = c.bufs(4, "hb")
        hls = [sb("hl%d" % i, [128, 1024], BF16, ph) for i in range(2)]; b_hls = c.bufs(2, "hl")
        hTls = [sb("hTl%d" % i, [128, 8, 128], BF16, ph) for i in range(2)]; b_hTls = c.bufs(2, "hTl")
        wrh = sb("wrh", [128, 8, 32], BF16, ph); wrl = sb("wrl", [128, 8, 32], BF16, ph)
        c.op("dve", lambda e: e.tensor_copy(out=wrh[:, :, :], in_=wr[:, :, :]), reads=[b_wr], writes=[b_wr])
        c.op("dve", lambda e: e.tensor_tensor(out=wrl[:, :, :], in0=wr[:, :, :], in1=wrh[:, :, :], op=ALU.subtract),
             reads=[b_wr], writes=[b_wr])
        hTb = [sb("hTb%d" % i, [128, 8, 128], BF16, ph) for i in range(2)]; b_hTb = c.bufs(2, "hTb")
        st = sb("st", [128, 2, 6], F32, ph); b_st = c.buf("st")
        mv = sb("mvst", [128, 4], F32, ph); b_mv = c.buf("mv")
        lg = sb("lg", [128, 32], F32, ph); b_lg = c.buf("lg")
        m8 = sb("m8", [128, 8], F32, ph); b_m8 = c.buf("m8")
        ix8 = sb("ix8", [128, 8], mybir.dt.uint32, ph); b_ix8 = c.buf("ix8")
        ixf = sb("ixf", [128, 8], F32, ph); b_ixf = c.buf("ixf")
        mskb = sb("mskb", [128, 32], BF16, ph); b_mskb = c.buf("mskb")
        ng = sb("ng", [128, 1], F32, ph); b_ng = c.buf("ng")
        e4 = sb("e4", [128, 4], F32, ph); b_e4 = c.buf("e4")
        ov = sb("ov", [128, 32], F32, ph); b_ov = c.buf("ov")
        dal = sb("dal", [128, 32], F32, ph); b_dal = c.buf("dal")
        d4f = sb("d4f", [128, 4], F32, ph); b_d4f = c.buf("d4f")
        oh4 = sb("oh4", [128, 4, 32], F32, ph); b_oh4 = c.buf("oh4")
        cm = sb("cm", [128, 32], F32, ph); b_cm = c.buf("cm")
        cmb = sb("cmb", [128, 32], BF16, ph); b_cmb = c.buf("cmb")
        c.op("dve", lambda e: e.memset(cm[:, :], 0.0), writes=[b_cm])
        c.op("dve", lambda e: e.memset(cmb[:, :], 0.0), writes=[b_cmb])
        c.op("dve", lambda e: e.memset(mskb[:, :], 0.0), writes=[b_mskb])
        ltri = sb("ltri_s", [128, 128], BF16, ph); b_ltri = c.buf("ltri")
        c.dma("pool", lambda q: q.dma_start(out=ltri[:, :], in_=ltri_d), writes=[b_ltri])
        iot = sb("iot", [128, 32], F32, ph); b_iot = c.buf("iot")
        c.dma("sp", lambda q: q.dma_start(out=iot[:, :], in_=iota_d), writes=[b_iot])
        ecs = sb("ecs_s", [128, 32], F32, ph); b_ecs = c.buf("ecs")
        c.dma("sp", lambda q: q.dma_start(out=ecs[:, :], in_=ecs_d), writes=[b_ecs])
        def P1(tt):
            rows = 128 if tt < 16 else NS
            ti = tt % 2
            tcs = slice(128 * tt, 128 * tt + rows)
            src = xtok[128 * tt:128 * tt + 128, :] if tt < 16 else xs[:, :]
            c.dma("sp", lambda q: q.dma_start(out=xk[ti][0:rows, :], in_=src), writes=[b_xk[ti]])
            yield
            for half in range(2):
                for k in range(8):
                    MM(PS[0:rows, half, :], uT[:, k, tcs], wo[:, k, 512 * half:512 * half + 512], k == 0, k == 7, [b_uT, b_wo], pb[half])
                    yield
                c.op("dve", lambda e: e.scalar_tensor_tensor(out=zz[ti][0:rows, 512 * half:512 * half + 512],
                                                             in0=xk[ti][0:rows, 512 * half:512 * half + 512], scalar=ALPHA,
                                                             in1=PS[0:rows, half, :], op0=ALU.mult, op1=ALU.add),
                     reads=[b_xk[ti], pb[half]], writes=[b_zz[ti]])
                yield
                c.op("dve", lambda e: e.bn_stats(out=st[0:rows, half, :], in_=zz[ti][0:rows, 512 * half:512 * half + 512]),
                     reads=[b_zz[ti]], writes=[b_st])
                yield
            c.op("dve", lambda e: e.bn_aggr(out=mv[0:rows, 0:2], in_=st[0:rows, :, :]), reads=[b_st], writes=[b_mv])
            yield
            c.op("dve", lambda e: e.tensor_scalar(out=mv[0:rows, 2:3], in0=mv[0:rows, 1:2], scalar1=LN_EPS, scalar2=None,
                                                  op0=ALU.add), reads=[b_mv], writes=[b_mv])
            yield
            c.op("act", lambda e: e.activation(out=mv[0:rows, 2:3], in_=mv[0:rows, 2:3], func=AF.Sqrt), reads=[b_mv], writes=[b_mv])
            yield
            c.op("dve", lambda e: e.reciprocal(out=mv[0:rows, 2:3], in_=mv[0:rows, 2:3]), reads=[b_mv], writes=[b_mv])
            yield
            c.op("dve", lambda e: e.scalar_tensor_tensor(out=mv[0:rows, 3:4], in0=mv[0:rows, 0:1], scalar=-1.0, in1=mv[0:rows, 2:3],
                                                         op0=ALU.mult, op1=ALU.mult), reads=[b_mv], writes=[b_mv])
            yield
            c.op("act", lambda e: e.activation(out=zz[ti][0:rows, :], in_=zz[ti][0:rows, :], func=AF.Identity,
                                               scale=mv[0:rows, 2:3], bias=mv[0:rows, 3:4]),
                 reads=[b_zz[ti], b_mv], writes=[b_zz[ti]])
            yield
            c.op("dve", lambda e: e.tensor_tensor(out=zz[ti][0:rows, :], in0=zz[ti][0:rows, :], in1=lnp[0:rows, 0, :], op=ALU.mult),
                 reads=[b_zz[ti], b_lnp], writes=[b_zz[ti]])
            yield
            c.op("dve", lambda e: e.tensor_tensor(out=zz[ti][0:rows, :], in0=zz[ti][0:rows, :], in1=lnp[0:rows, 1, :], op=ALU.add),
                 reads=[b_zz[ti], b_lnp], writes=[b_zz[ti]])
            yield
            c.op("act", lambda e: e.activation(out=fa[ti][0:rows, :], in_=zz[ti][0:rows, :], func=AF.Copy, scale=ALPHA),
                 reads=[b_zz[ti]], writes=[b_fa[ti]])
            yield
            c.dma("sp", lambda q: q.dma_start(out=fa_d.ap()[tt, 0:rows, :], in_=fa[ti][0:rows, :]), reads=[b_fa[ti]],
                  writes=[b_fad], own=b_fad)
            yield
            c.op("act", lambda e: e.activation(out=hbs[tt % 4][0:rows, :], in_=zz[ti][0:rows, :], func=AF.Copy), reads=[b_zz[ti]], writes=[b_hbs[tt % 4]])
            yield
            c.op("dve", lambda e: e.tensor_tensor(out=hls[ti][0:rows, :], in0=zz[ti][0:rows, :], in1=hbs[tt % 4][0:rows, :], op=ALU.subtract),
                 reads=[b_zz[ti], b_hbs[tt % 4]], writes=[b_hls[ti]])
            yield
        def P2(tt):
            rows = 128 if tt < 16 else NS
            ti = tt % 2
            hl, b_hl, hTl, b_hTl = hls[ti], b_hls[ti], hTls[ti], b_hTls[ti]
            for part, (srcb, bsrc) in enumerate(((hbs[tt % 4], b_hbs[tt % 4]), (hl, b_hl))):
                pv = PS[:, 2 + part, :].bitcast(BF16)
                for k in range(8):
                    c.op("pe", lambda e: e.transpose(out=pv[:, 128 * k:128 * k + rows], in_=srcb[0:rows, 128 * k:128 * k + 128],
                                                     identity=ident_b[0:rows, 0:rows]),
                         reads=[bsrc, b_ident], writes=[pb[2 + part]])
                    yield
            pv0 = PS[:, 2, :].bitcast(BF16).rearrange("p (k n) -> p k n", n=128)[:, :, 0:rows]
            pv1 = PS[:, 3, :].bitcast(BF16).rearrange("p (k n) -> p k n", n=128)[:, :, 0:rows]
            c.op("act", lambda e: e.activation(out=hTb[ti][:, :, 0:rows], in_=pv0, func=AF.Copy), reads=[pb[2]], writes=[b_hTb[ti]])
            yield
            c.op("dve", lambda e: e.tensor_copy(out=hTl[:, :, 0:rows], in_=pv1), reads=[pb[3]], writes=[b_hTl])
            yield
            nmm = 0
            for k in range(8):
                for (lt, bl, rt) in ((hTb[ti], b_hTb[ti], wrh), (hTl, b_hTl, wrh), (hTb[ti], b_hTb[ti], wrl)):
                    MM(PS[0:rows, 4, 0:32], lt[:, k, 0:rows], rt[:, k, :], nmm == 0, nmm == 23, [bl, b_wr], pb[4])
                    yield
                    nmm += 1
            c.op("dve", lambda e: e.tensor_tensor(out=lg[0:rows, :], in0=PS[0:rows, 4, 0:32], in1=brt[0:rows, :], op=ALU.add),
                 reads=[pb[4], b_lnp], writes=[b_lg])
            yield
            c.op("dve", lambda e: e.max(out=m8[0:rows, :], in_=lg[0:rows, :]), reads=[b_lg], writes=[b_m8])
            yield
            c.op("dve", lambda e: e.max_index(out=ix8[0:rows, :], in_max=m8[0:rows, :], in_values=lg[0:rows, :]),
                 reads=[b_lg, b_m8], writes=[b_ix8])
            yield
            c.op("dve", lambda e: e.tensor_copy(out=ixf[0:rows, :], in_=ix8[0:rows, :]), reads=[b_ix8], writes=[b_ixf])
            yield
            if tt == 16:
                c.op("dve", lambda e: e.memset(mskb[:, :], 0.0), writes=[b_mskb])
                yield
            c.op("dve", lambda e: e.tensor_scalar(out=mskb[0:rows, :], in0=lg[0:rows, :], scalar1=m8[0:rows, 3:4], scalar2=None,
                                                  op0=ALU.is_ge), reads=[b_lg, b_m8], writes=[b_mskb])
            yield
            c.op("dve", lambda e: e.tensor_scalar(out=ng[0:rows, :], in0=m8[0:rows, 0:1], scalar1=-1.0, scalar2=None, op0=ALU.mult),
                 reads=[b_m8], writes=[b_ng])
            yield
            c.op("act", lambda e: e.activation(out=e4[0:rows, :], in_=m8[0:rows, 0:4], func=AF.Exp, bias=ng[0:rows, 0:1]),
                 reads=[b_m8, b_ng], writes=[b_e4])
            yield
            c.op("dve", lambda e: e.reduce_sum(out=ng[0:rows, :], in_=e4[0:rows, :], axis=AX.X), reads=[b_e4, b_ng], writes=[b_ng])
            yield
            c.op("dve", lambda e: e.reciprocal(out=ng[0:rows, :], in_=ng[0:rows, :]), reads=[b_ng], writes=[b_ng])
            yield
            c.op("dve", lambda e: e.tensor_scalar(out=gates4[0:rows, tt, :], in0=e4[0:rows, :], scalar1=ng[0:rows, 0:1], scalar2=None,
                                                  op0=ALU.mult), reads=[b_e4, b_ng], writes=[b_gates4])
            yield
            MM(PS[0:rows, 5, 0:32], ltri[0:rows, 0:rows], mskb[0:rows, :], True, False, [b_ltri, b_mskb], pb[5])
            yield
            MM(PS[0:rows, 5, 0:32], ones_b[:, 0:rows], cmb[:, :], False, True, [b_ones, b_cmb], pb[5])
            yield
            c.op("dve", lambda e: e.tensor_scalar(out=ov[0:rows, :], in0=PS[0:rows, 5, 0:32], scalar1=float(CAP) - 0.5, scalar2=1.0e6,
                                                  op0=ALU.is_ge, op1=ALU.mult), reads=[pb[5]], writes=[b_ov])
            yield
            c.op("dve", lambda e: e.tensor_tensor(out=dal[0:rows, :], in0=PS[0:rows, 5, 0:32], in1=ecs[0:rows, :], op=ALU.add),
                 reads=[pb[5], b_ecs], writes=[b_dal])
            yield
            c.op("dve", lambda e: e.tensor_tensor(out=dal[0:rows, :], in0=dal[0:rows, :], in1=ov[0:rows, :], op=ALU.add),
                 reads=[b_dal, b_ov], writes=[b_dal])
            yield
            c.op("dve", lambda e: e.tensor_tensor(out=cm[:, :], in0=cm[:, :], in1=mskb[:, :], op=ALU.add), reads=[b_cm, b_mskb], writes=[b_cm])
            yield
            c.op("dve", lambda e: e.tensor_copy(out=cmb[:, :], in_=cm[:, :]), reads=[b_cm], writes=[b_cmb])
            yield
            c.op("dve", lambda e: e.tensor_tensor(out=oh4[0:rows, :, :], in0=iot[0:rows, :].unsqueeze(1).to_broadcast([rows, 4, 32]),
                                                  in1=ixf[0:rows, 0:4].unsqueeze(2).to_broadcast([rows, 4, 32]), op=ALU.is_equal),
                 reads=[b_iot, b_ixf], writes=[b_oh4])
            yield
            c.op("dve", lambda e: e.tensor_tensor(out=oh4[0:rows, :, :], in0=oh4[0:rows, :, :],
                                                  in1=dal[0:rows, :].unsqueeze(1).to_broadcast([rows, 4, 32]), op=ALU.mult),
                 reads=[b_oh4, b_dal], writes=[b_oh4])
            yield
            c.op("dve", lambda e: e.reduce_sum(out=d4f[0:rows, :], in_=oh4[0:rows, :, :], axis=AX.X), reads=[b_oh4], writes=[b_d4f])
            yield
            c.op("dve", lambda e: e.tensor_copy(out=dest4[0:rows, tt, :], in_=d4f[0:rows, :]), reads=[b_d4f], writes=[b_dest4])
            yield
            for k in range(4):
                c.dma("pool", lambda q: q.indirect_dma_start(
                    out=Xd.ap(), out_offset=bass.IndirectOffsetOnAxis(ap=dest4[:, tt, k:k + 1], axis=0),
                    in_=hbs[tt % 4][:, :], in_offset=None, bounds_check=bcreg, oob_is_err=False),
                    reads=[b_hbs[tt % 4], b_dest4], writes=[b_Xd], own=b_Xd)
                yield
        def rr(*gens):
            gens = [g for g in gens if g is not None]
            while gens:
                for g in list(gens):
                    try:
                        next(g)
                    except StopIteration:
                        gens.remove(g)

        rr(P1(0))
        for tt in range(17):
            rr(P1(tt + 1) if tt + 1 < 17 else None, P2(tt))
        c.barrier()
        if stop == "WO":
            c.finish("sp")
            return nc

    with ExitStack() as ph:
        bgu = sb("bgu", [128, 32, 16], F32, ph); b_bgu = c.buf("bgu")
        c.dma("sp", lambda q: q.dma_start(out=bgu[:, :, :], in_=b_gu), writes=[b_bgu])
        bgu7 = sb("bgu7", [128, 32, 8], F32, ph)
        c.op("dve", lambda e: e.tensor_scalar(out=bgu7[:, :, :], in0=bgu[:, :, 8:16], scalar1=7.0, scalar2=None, op0=ALU.add),
             reads=[b_bgu], writes=[b_bgu])
        Xe = [sb("Xe%d" % i, [128, 3, 1024], BF16, ph) for i in range(2)]; b_Xe = c.bufs(2, "Xe")
        XeT = [sb("XeT%d" % i, [128, 8, CAP], BF16, ph) for i in range(2)]; b_XeT = c.bufs(2, "XeT")
        hmid = [sb("hmid%d" % i, [128, 8, CAP], BF16, ph) for i in range(2)]; b_hmid = c.bufs(2, "hmid")
        tg_ = [sb("tgg%d" % i, [128, CAP], F32, ph) for i in range(2)]; b_tg = c.bufs(2, "tgg")
        ts_ = [sb("tss%d" % i, [128, CAP], F32, ph) for i in range(2)]; b_ts = c.bufs(2, "tss")
        tu_ = [sb("tuu%d" % i, [128, CAP], F32, ph) for i in range(2)]; b_tu = c.bufs(2, "tuu")
        Yo = [sb("Yo%d" % i, [128, 1024], F32, ph) for i in range(3)]; b_Yo = c.bufs(3, "Yo")
        nyo = [0]

        def load_X(e_):
            bi = e_ % 2
            c.dma("sp", lambda q: q.dma_start(out=Xe[bi][:, :, :], in_=Xd.ap()[e_ * CAP:(e_ + 1) * CAP, :].rearrange("(b p) n -> p b n", p=128)),
                  reads=[b_Xd], writes=[b_Xe[bi]])

        def emit_T(e_):
            bi = e_ % 2
            for blk in range(3):
                tb = blk % 2
                pv = PS[:, tb, :].bitcast(BF16)
                for k in range(8):
                    c.op("pe", lambda e: e.transpose(out=pv[:, 128 * k:128 * k + 128], in_=Xe[bi][:, blk, 128 * k:128 * k + 128],
                                                     identity=ident_b[:, :]), reads=[b_Xe[bi], b_ident], writes=[pb[tb]])
                EV(XeT[bi][:, :, 128 * blk:128 * blk + 128], pv.rearrange("p (k n) -> p k n", n=128), [pb[tb]], [b_XeT[bi]])

        def stage2(e_, fc):
            bi, ti = e_ % 2, fc % 2
            c.op("dve", lambda e: e.tensor_scalar(out=tu_[ti][:, :], in0=tu_[ti][:, :], scalar1=14.0, scalar2=-6.0,
                                                  op0=ALU.min, op1=ALU.add), reads=[b_tu[ti]], writes=[b_tu[ti]])
            c.op("dve", lambda e: e.tensor_tensor(out=tg_[ti][:, :], in0=tg_[ti][:, :], in1=ts_[ti][:, :], op=ALU.mult),
                 reads=[b_tg[ti], b_ts[ti]], writes=[b_tg[ti]])
            c.op("dve", lambda e: e.tensor_tensor(out=hmid[bi][:, fc, :], in0=tg_[ti][:, :], in1=tu_[ti][:, :], op=ALU.mult),
                 reads=[b_tg[ti], b_tu[ti]], writes=[b_hmid[bi]])

        def emit_GU(e_):
            bi = e_ % 2
            for fc in range(8):
                sl = piece_slot[6 * e_ + fc // 2]
                sub = fc % 2
                gb, ub, ti = 2 + fc % 2, 4 + fc % 2, fc % 2
                for k in range(8):
                    MM(PS[:, gb, 0:CAP], wp[sl][:, k, 128 * sub:128 * sub + 128], XeT[bi][:, k, :], k == 0, k == 7,
                       [b_wp[sl], b_XeT[bi]], pb[gb])
                for k in range(8):
                    MM(PS[:, ub, 0:CAP], wp[sl][:, k, 256 + 128 * sub:256 + 128 * sub + 128], XeT[bi][:, k, :], k == 0, k == 7,
                       [b_wp[sl], b_XeT[bi]], pb[ub])
                if fc >= 1:
                    stage2(e_, fc - 1)
                c.op("dve", lambda e: e.tensor_scalar(out=tg_[ti][:, :], in0=PS[:, gb, 0:CAP], scalar1=bgu[:, e_, fc:fc + 1],
                                                      scalar2=7.0, op0=ALU.add, op1=ALU.min),
                     reads=[pb[gb], b_bgu], writes=[b_tg[ti]])
                c.op("act", lambda e: e.activation(out=ts_[ti][:, :], in_=tg_[ti][:, :], func=AF.Sigmoid, scale=1.702),
                     reads=[b_tg[ti]], writes=[b_ts[ti]])
                c.op("act", lambda e: e.activation(out=tu_[ti][:, :], in_=PS[:, ub, 0:CAP], func=AF.Relu, bias=bgu7[:, e_, fc:fc + 1]),
                     reads=[pb[ub], b_bgu], writes=[b_tu[ti]])
            stage2(e_, 7)

        def emit_D(e_):
            bi = e_ % 2
            for blk in range(3):
                yi = nyo[0] % 3
                nyo[0] += 1
                for half in range(2):
                    db_ = 6 + half
                    sl = piece_slot[6 * e_ + 4 + half]
                    for f in range(8):
                        MM(PS[:, db_, :], hmid[bi][:, f, 128 * blk:128 * blk + 128], wp[sl][:, f, :], f == 0, False,
                           [b_hmid[bi], b_wp[sl]], pb[db_])
                    MM(PS[:, db_, :], ones_b[0:1, :], bdn[e_ % 4][0:1, 512 * half:512 * half + 512], False, True,
                       [b_ones, b_bdn[e_ % 4]], pb[db_])
                    EV(Yo[yi][:, 512 * half:512 * half + 512], PS[:, db_, :], [pb[db_]], [b_Yo[yi]])
                r0 = e_ * CAP + 128 * blk
                c.dma("sp", lambda q: q.dma_start(out=Yd.ap()[r0:r0 + 128, :], in_=Yo[yi][:, :]), reads=[b_Yo[yi]],
                      writes=[b_Yd], own=b_Yd)

        load_X(0)
        emit_T(0)
        for e_ in range(n_experts):
            def top_up(limit):
                while npiece_issued[0] < min(limit, 6 * n_experts):
                    issue_piece()
            if e_ + 1 < n_experts:
                load_X(e_ + 1)
            top_up(6 * e_ + NR)
            emit_GU(e_)
            top_up(6 * e_ + NR + 4)
            if e_ + 1 < n_experts:
                emit_T(e_ + 1)
            emit_D(e_)
            top_up(6 * e_ + NR + 6)
        c.barrier()
    with ExitStack() as ph:
        lnp2 = sb("lnp2", [128, 2, 1024], F32, ph); b_lnp2 = c.buf("lnp2")
        c.dma("sp", lambda q: q.dma_start(out=lnp2[:, 0, :], in_=AP(ln2_g.tensor, 0, [[0, 128], [1, 1024]])), writes=[b_lnp2], own=b_lnp2)
        c.dma("sp", lambda q: q.dma_start(out=lnp2[:, 1, :], in_=AP(ln2_b.tensor, 0, [[0, 128], [1, 1024]])), writes=[b_lnp2], own=b_lnp2)
        Gk = [[sb("Gk%d_%d" % (i, k), [128, 1024], F32, ph) for k in range(4)] for i in range(2)]
        b_Gk = [[c.buf("Gk%d_%d" % (i, k)) for k in range(4)] for i in range(2)]
        fac = [sb("fac%d" % i, [128, 1024], F32, ph) for i in range(2)]; b_fac = c.bufs(2, "fac")
        st2s = [sb("st2_%d" % i, [128, 2, 6], F32, ph) for i in range(2)]; b_st2s = c.bufs(2, "st2")
        mv2s = [sb("mv2_%d" % i, [128, 4], F32, ph) for i in range(2)]; b_mv2s = c.bufs(2, "mv2")
        def CB(tt):
            rows = 128 if tt < 16 else NS
            ti = tt % 2
            c.dma("sp", lambda q: q.dma_start(out=fac[ti][0:rows, :], in_=fa_d.ap()[tt, 0:rows, :]), reads=[b_fad], writes=[b_fac[ti]])
            yield
            for k in range(4):
                c.dma("pool", lambda q: q.indirect_dma_start(
                    out=Gk[ti][k][:, :], out_offset=None, in_=Yd.ap(),
                    in_offset=bass.IndirectOffsetOnAxis(ap=dest4[:, tt, k:k + 1], axis=0),
                    bounds_check=bcreg, oob_is_err=False),
                    reads=[b_Yd, b_dest4], writes=[b_Gk[ti][k]])
                yield
            for k in range(4):
                c.op("dve", lambda e: e.scalar_tensor_tensor(out=fac[ti][0:rows, :], in0=Gk[ti][k][0:rows, :],
                                                             scalar=gates4[0:rows, tt, k:k + 1], in1=fac[ti][0:rows, :],
                                                             op0=ALU.mult, op1=ALU.add),
                     reads=[b_Gk[ti][k], b_gates4, b_fac[ti]], writes=[b_fac[ti]])
                yield
            for half in range(2):
                c.op("dve", lambda e: e.bn_stats(out=st2s[ti][0:rows, half, :], in_=fac[ti][0:rows, 512 * half:512 * half + 512]),
                     reads=[b_fac[ti]], writes=[b_st2s[ti]])
                yield
            c.op("dve", lambda e: e.bn_aggr(out=mv2s[ti][0:rows, 0:2], in_=st2s[ti][0:rows, :, :]), reads=[b_st2s[ti]], writes=[b_mv2s[ti]])
            yield
            c.op("dve", lambda e: e.tensor_scalar(out=mv2s[ti][0:rows, 2:3], in0=mv2s[ti][0:rows, 1:2], scalar1=LN_EPS, scalar2=None,
                                                  op0=ALU.add), reads=[b_mv2s[ti]], writes=[b_mv2s[ti]])
            yield
            c.op("act", lambda e: e.activation(out=mv2s[ti][0:rows, 2:3], in_=mv2s[ti][0:rows, 2:3], func=AF.Sqrt), reads=[b_mv2s[ti]], writes=[b_mv2s[ti]])
            yield
            c.op("dve", lambda e: e.reciprocal(out=mv2s[ti][0:rows, 2:3], in_=mv2s[ti][0:rows, 2:3]), reads=[b_mv2s[ti]], writes=[b_mv2s[ti]])
            yield
            c.op("dve", lambda e: e.scalar_tensor_tensor(out=mv2s[ti][0:rows, 3:4], in0=mv2s[ti][0:rows, 0:1], scalar=-1.0, in1=mv2s[ti][0:rows, 2:3],
                                                         op0=ALU.mult, op1=ALU.mult), reads=[b_mv2s[ti]], writes=[b_mv2s[ti]])
            yield
            c.op("act", lambda e: e.activation(out=fac[ti][0:rows, :], in_=fac[ti][0:rows, :], func=AF.Identity,
                                               scale=mv2s[ti][0:rows, 2:3], bias=mv2s[ti][0:rows, 3:4]),
                 reads=[b_fac[ti], b_mv2s[ti]], writes=[b_fac[ti]])
            yield
            c.op("dve", lambda e: e.tensor_tensor(out=fac[ti][0:rows, :], in0=fac[ti][0:rows, :], in1=lnp2[0:rows, 0, :], op=ALU.mult),
                 reads=[b_fac[ti], b_lnp2], writes=[b_fac[ti]])
            yield
            c.op("dve", lambda e: e.tensor_tensor(out=fac[ti][0:rows, :], in0=fac[ti][0:rows, :], in1=lnp2[0:rows, 1, :], op=ALU.add),
                 reads=[b_fac[ti], b_lnp2], writes=[b_fac[ti]])
            yield
            dst = y_o[128 * tt:128 * tt + 128, :] if tt < 16 else ys_o[:, :]
            c.dma("sp", lambda q: q.dma_start(out=dst, in_=fac[ti][0:rows, :]), reads=[b_fac[ti]], own=b_fac[ti], final=True)
            yield
        def rr2(*gens):
            gens = [g for g in gens if g is not None]
            while gens:
                for g in list(gens):
                    try:
                        next(g)
                    except StopIteration:
                        gens.remove(g)

        for tt in range(0, 17, 2):
            rr2(CB(tt), CB(tt + 1) if tt + 1 < 17 else None)
        c.finish("sp")
    es.close()
    return nc


def _t5_bucket_np(d):
    d = np.maximum(d, 0)
    dl = np.maximum(d, 16).astype(np.float32)
    large = 16 + (np.log(dl / np.float32(16)) / np.float32(math.log(2048 / 16)) * np.float32(16)).astype(np.int32)
    return np.where(d < 16, d, np.minimum(large, 31))


def _make_oh():
    oh = np.zeros((33, 4, FVL), np.float32)
    for s, (dil, wmax) in enumerate(((1, 127), (1, 128), (4, 128), (16, 128))):
        for m in range(FVL):
            dist = m - 127
            if 0 <= dist <= wmax:
                oh[int(_t5_bucket_np(np.array(dist * dil))), s, m] = 1.0
            else:
                oh[32, s, m] = NEG
    return oh


_NC_CACHE = {}
_PREP_ONLY = False


def kernel(x_prompt, x_sample, cache_a_k, cache_a_v, cache_b_k, cache_b_v, cache_mem_k, cache_mem_v,
           mem_prompt, rel_bias, sinks_a, w_in, w_mem_kv, w_br_a, w_br_b, w_br_m, w_o,
           ln1_g, ln1_b, ln2_g, ln2_b, w_router, b_router, w_gu, b_gu, w_down, b_down):
    f32 = np.float32
    A = lambda a: np.ascontiguousarray(np.asarray(a, dtype=f32))
    xp = np.asarray(x_prompt, f32)[0]
    xpad = np.concatenate([np.zeros((2048, 1024), f32), xp], axis=0)
    shared = {
        "memT": A(np.asarray(mem_prompt, f32)[0].T), "rel_bias": A(rel_bias), "sinks": A(sinks_a),
        "oh": _make_oh(), "ident": np.eye(128, dtype=f32),
        "ltri": np.triu(np.ones((128, 128), f32), 1),
        "iota_e": np.tile(np.arange(32, dtype=f32)[None, :], (128, 1)),
        "ecs": np.tile((np.arange(32, dtype=f32) * CAP)[None, :], (128, 1)),
        "w_in": A(np.asarray(w_in)[0]), "w_mem_kv": A(np.asarray(w_mem_kv)[0]), "w_br_a": A(np.asarray(w_br_a)[0]),
        "w_br_b": A(np.asarray(w_br_b)[0]), "w_br_m": A(np.asarray(w_br_m)[0]), "w_o": A(np.asarray(w_o)[0]),
        "ln1_g": A(ln1_g), "ln1_b": A(ln1_b), "ln2_g": A(ln2_g), "ln2_b": A(ln2_b),
        "w_router": A(np.asarray(w_router)[0]), "b_router": A(b_router),
        "w_gu": A(np.asarray(w_gu, f32)[0].reshape(32, 8, 128, 2, 4, 256).transpose(0, 4, 2, 1, 3, 5).reshape(32, 4, 128, 4096)), "b_gu": A(np.asarray(b_gu, f32)[0].reshape(32, 16, 128).transpose(2, 0, 1)),
        "w_down": A(np.asarray(w_down, f32)[0].reshape(32, 8, 128, 2, 512).transpose(0, 3, 2, 1, 4).reshape(32, 2, 128, 4096)), "b_down": A(np.asarray(b_down)[0]),
    }
    cak = np.asarray(cache_a_k, f32)[0]; cavv = np.asarray(cache_a_v, f32)[0]
    cbk = np.asarray(cache_b_k, f32)[0]; cbvv = np.asarray(cache_b_v, f32)[0]
    cmk = np.asarray(cache_mem_k, f32)[0]; cmvv = np.asarray(cache_mem_v, f32)[0]
    xsm = np.asarray(x_sample, f32)
    kk = np.arange(128)
    rows = [1920 + kk]
    for d in (4, 16):
        for i in range(4):
            rows.append(2048 + i - d * (128 - kk))
    rows = np.stack(rows)
    in_maps = []
    for cidx in range(8):
        s0 = 2048 * cidx
        base = s0 + 2048
        i128 = np.arange(128)
        idx0 = np.arange(base - 128, base + 2048)
        idx1 = np.concatenate([base - 512 + 4 * i128 + r for r in range(4)] +
                              [base + 4 * (128 * m + i128) + r for r in range(4) for m in range(4)])
        idx2 = np.concatenate([base - 2048 + 16 * i128 + r for r in range(16)] +
                              [base + 16 * i128 + r for r in range(16)])
        bs = slice(16 * cidx, 16 * cidx + 16)
        m = dict(shared)
        m["xt0"] = A(xpad[idx0].T); m["xt1"] = A(xpad[idx1].T); m["xt2"] = A(xpad[idx2].T)
        m["xtok"] = A(xp[s0:s0 + 2048])
        xs_ = xsm[bs].reshape(64, 1024)
        m["xst"] = A(xs_.T); m["xs"] = A(xs_)
        m["cakT"] = A(cak[bs].transpose(3, 0, 2, 1))
        m["cav"] = A(cavv[bs].transpose(1, 0, 2, 3).reshape(128, 16, 128))
        kb = cbk[bs][:, rows]
        m["cbkT"] = A(kb.transpose(0, 4, 1, 3, 2).reshape(16, 64, 9 * 4 * 128))
        vb = cbvv[bs][:, rows]
        m["cbv"] = A(vb.transpose(0, 2, 1, 3, 4).reshape(16, 128, 9 * 4 * 64))
        m["cmkT"] = A(cmk[bs].transpose(0, 3, 2, 1).reshape(16, 128, 4 * 256))
        m["cmv"] = A(cmvv[bs].reshape(16, 2, 128, 512).transpose(0, 2, 1, 3).reshape(16, 128, 1024))
        m["hmask"] = np.full((128, 1), NEG if cidx == 0 else 0.0, f32)
        in_maps.append(m)
    if _PREP_ONLY:
        return in_maps
    if "nc" not in _NC_CACHE:
        _NC_CACHE["nc"] = build()
    res = run_bass_kernel_spmd(_NC_CACHE["nc"], in_maps, core_ids=list(range(8)))
    R = res.results
    y_prompt = np.concatenate([r["y"] for r in R], axis=0).reshape(1, 16384, 1024)
    y_sample = np.concatenate([r["ys"] for r in R], axis=0).reshape(128, 4, 1024)
    last = R[7]
    a_k = last["ak"].reshape(1, 1, 128, 2, 64); a_v = last["av"].reshape(1, 1, 128, 2, 64)
    b_k = last["bk"].reshape(1, 1, 2048, 4, 64); b_v = last["bv"].reshape(1, 1, 2048, 4, 64)
    m_k = R[0]["mk"].reshape(1, 1, 256, 4, 128); m_v = R[0]["mv"].reshape(1, 1, 256, 4, 128)
    cat = lambda n, w: np.concatenate([r[n] for r in R], axis=0)
    sa_k = cat("sak", 128).reshape(1, 128, 4, 2, 64); sa_v = cat("sav", 128).reshape(1, 128, 4, 2, 64)
    sb_k = cat("sbk", 256).reshape(1, 128, 4, 4, 64); sb_v = cat("sbv", 256).reshape(1, 128, 4, 4, 64)
    return tuple(np.asarray(a, dtype=np.float32) for a in
                 (y_prompt, y_sample, a_k, a_v, b_k, b_v, m_k, m_v, sa_k, sa_v, sb_k, sb_v))
```

```python
import math
from contextlib import ExitStack
import numpy as np
import concourse.bass as bass
import concourse.mybir as mybir
from concourse.bass_utils import run_bass_kernel_spmd

F32 = mybir.dt.float32
BF16 = mybir.dt.bfloat16
I32 = mybir.dt.int32
ALU = mybir.AluOpType
AF = mybir.ActivationFunctionType
AX = mybir.AxisListType


class Buf:
    __slots__ = ("name", "w", "r", "dsem", "dval")

    def __init__(self, name):
        self.name = name
        self.w = None
        self.r = {}
        self.dsem = None
        self.dval = 0


class Ctx:
    def __init__(self, nc):
        self.nc = nc
        self.engs = {"pe": nc.tensor, "act": nc.scalar, "dve": nc.vector,
                     "pool": nc.gpsimd, "sp": nc.sync}
        self.esem = {k: nc.alloc_semaphore("es_" + k) for k in ("pe", "act", "dve", "pool")}
        self.ecnt = {k: 0 for k in self.esem}
        self.seen = {k: {} for k in self.engs}
        self.nbuf = 0
        self.skip_own = {"pe"}
        self.all_dma = []
        self.dma_bufs = []

    def buf(self, name=None):
        self.nbuf += 1
        return Buf(name or ("b%d" % self.nbuf))

    def bufs(self, n, name="b"):
        return [self.buf("%s%d" % (name, i)) for i in range(n)]

    def _wait(self, e, sem, val):
        if sem is None:
            return
        seen = self.seen[e]
        if seen.get(sem, 0) >= val:
            return
        if e in self.skip_own and sem is self.esem.get(e):
            return
        self.engs[e].wait_ge(sem, val)
        seen[sem] = val

    def _deps(self, e, reads, writes):
        own = self.esem.get(e)
        for b in reads:
            if b.w is not None:
                self._wait(e, *b.w)
        for b in writes:
            if b.w is not None:
                self._wait(e, *b.w)
            for s, v in b.r.items():
                self._wait(e, s, v)

    def op(self, e, fn, reads=(), writes=(), inc=True):
        self._deps(e, reads, writes)
        ins = fn(self.engs[e])
        sem = self.esem[e]
        if inc:
            self.ecnt[e] += 1
            v = self.ecnt[e]
            ins.then_inc(sem, 1)
        else:
            v = self.ecnt[e] + 1
        for b in reads:
            if b.r.get(sem, 0) < v:
                b.r[sem] = v
        for b in writes:
            b.w = (sem, v)
            b.r = {}
        return ins

    def dma(self, q, fn, reads=(), writes=(), own=None, final=False):
        self._deps(q, reads, writes)
        if own is None:
            own = writes[0] if writes else reads[0]
        if own.dsem is None:
            own.dsem = self.nc.alloc_semaphore("ds_" + own.name)
            self.dma_bufs.append(own)
        ins = fn(self.engs[q])
        own.dval += 16
        ins.then_inc(own.dsem, 16)
        ev = (own.dsem, own.dval)
        for b in reads:
            if b.r.get(ev[0], 0) < ev[1]:
                b.r[ev[0]] = ev[1]
        for b in writes:
            b.w = ev
            b.r = {}
        if final:
            self.all_dma.append(ev)
        return ins

    def barrier(self, engines=("pe", "act", "dve", "pool", "sp")):
        for e in engines:
            for k, sem in self.esem.items():
                if k != e and self.ecnt[k] > 0:
                    self._wait(e, sem, self.ecnt[k])
            for b in self.dma_bufs:
                if b.dval > 0:
                    self._wait(e, b.dsem, b.dval)

    def finish(self, e="sp"):
        for sem, val in self.all_dma:
            self._wait(e, sem, val)


NT = 2048
NS = 64
NTOK = NT + NS
NB = 16
ALPHA = float(2.0 ** 0.25)
NEG = -30000.0
LN_EPS = 1e-5
NH_G = (1, 4, 16)
FVL = 384
FVR = 129
DEBUG_OUT = False
CAP = 384
NRX = 32 * CAP


def build(n_experts=32, stop=None):
    nc = bass.Bass("TRN2", target_bir_lowering=False)
    c = Ctx(nc)

    def din(n, s, dt=F32):
        return nc.dram_tensor(n, list(s), dt, kind="ExternalInput").ap()

    def dout(n, s, dt=F32):
        return nc.dram_tensor(n, list(s), dt, kind="ExternalOutput").ap()

    xt = [din("xt0", [1024, 17 * 128]), din("xt1", [1024, 20 * 128]), din("xt2", [1024, 32 * 128])]
    xtok = din("xtok", [NT, 1024])
    xst = din("xst", [1024, NS])
    xs = din("xs", [NS, 1024])
    memT = din("memT", [1024, 256])
    cakT = din("cakT", [64, NB, 2, 128])
    cav = din("cav", [128, NB, 128])
    cbkT = din("cbkT", [NB, 64, 9 * 4 * 128])
    cbv = din("cbv", [NB, 128, 9 * 4 * 64])
    cmkT = din("cmkT", [NB, 128, 4 * 256])
    cmv = din("cmv", [NB, 128, 2 * 512])
    rel_bias = din("rel_bias", [32, 20])
    sinks = din("sinks", [1, 8])
    oh = din("oh", [33, 4, FVL])
    hmask = din("hmask", [128, 1])
    ident = din("ident", [128, 128])
    w_in = din("w_in", [1024, 5632])
    w_mem_kv = din("w_mem_kv", [1024, 1024])
    w_br_a = din("w_br_a", [512, 1024])
    w_br_b = din("w_br_b", [256, 1024])
    w_br_m = din("w_br_m", [512, 1024])
    w_o = din("w_o", [1024, 1024])
    ln1_g = din("ln1_g", [1, 1024]); ln1_b = din("ln1_b", [1, 1024])
    ln2_g = din("ln2_g", [1, 1024]); ln2_b = din("ln2_b", [1, 1024])
    w_router = din("w_router", [1024, 32]); b_router = din("b_router", [1, 32])
    w_gu = din("w_gu", [32, 4, 128, 4096]); b_gu = din("b_gu", [128, 32, 16])
    w_down = din("w_down", [32, 2, 128, 4096]); b_down = din("b_down", [32, 1024])

    y_o = dout("y", [NT, 1024]); ys_o = dout("ys", [NS, 1024])
    ak_o = dout("ak", [128, 128]); av_o = dout("av", [128, 128])
    bk_o = dout("bk", [NT, 256]); bv_o = dout("bv", [NT, 256])
    mk_o = dout("mk", [256, 512]); mv_o = dout("mv", [256, 512])
    sak_o = dout("sak", [NS, 128]); sav_o = dout("sav", [NS, 128])
    sbk_o = dout("sbk", [NS, 256]); sbv_o = dout("sbv", [NS, 256])

    ltri_d = din("ltri", [128, 128]); iota_d = din("iota_e", [128, 32]); ecs_d = din("ecs", [128, 32])
    Xd = nc.dram_tensor("Xd", [NRX, 1024], BF16, kind="Internal"); b_Xd = c.buf("Xd")
    Yd = nc.dram_tensor("Yd", [NRX, 1024], F32, kind="Internal"); b_Yd = c.buf("Yd")
    fvd = nc.dram_tensor("fvd", [20, FVR, FVL], BF16, kind="Internal")
    vnd = nc.dram_tensor("vnd", [NS, 384], BF16, kind="Internal")
    b_fvd = c.buf("fvd"); b_vnd = c.buf("vnd")

    bcreg = nc.gpsimd.alloc_register("bcreg")
    nc.gpsimd.reg_mov(bcreg, NRX - 1)
    PS = nc.alloc_psum_tensor("ps", [128, 8, 512], F32)
    pb = c.bufs(8, "psb")

    def MM(out, lhsT, rhs, start, stop, rd, wr):
        c.op("pe", lambda e: e.matmul(out, lhsT=lhsT, rhs=rhs, start=start, stop=stop),
             reads=rd, writes=[wr], inc=stop)

    evc = [0]

    def EV(out, in_, rd, wr, scale=None, eng=None):
        if eng is None:
            eng = ("act", "dve")[evc[0] % 2]
            evc[0] += 1
        if eng == "act":
            c.op("act", lambda e: e.activation(out=out, in_=in_, func=AF.Copy,
                                               scale=(1.0 if scale is None else scale)),
                 reads=rd, writes=wr)
        else:
            if scale is None:
                c.op(eng, lambda e: e.tensor_copy(out=out, in_=in_), reads=rd, writes=wr)
            else:
                c.op(eng, lambda e: e.tensor_scalar(out=out, in0=in_, scalar1=float(scale), scalar2=None,
                                                    op0=ALU.mult), reads=rd, writes=wr)

    def AP(t, off, dims):
        return bass.AP(tensor=t, offset=off, ap=[list(d) for d in dims])

    es = ExitStack()
    es2 = ExitStack()

    def sb(name, shape, dt, stack=None):
        return (stack or es).enter_context(nc.sbuf_tensor(name, list(shape), dt))

    ident_b = sb("ident_b", [128, 128], BF16); b_ident = c.buf("ident")
    ident_f = sb("ident_f", [128, 128], F32); b_identf = c.buf("identf")
    ones_b = sb("ones_b", [128, 128], BF16); b_ones = c.buf("ones")
    c.dma("pool", lambda q: q.dma_start(out=ident_b[:, :], in_=ident), writes=[b_ident])
    c.dma("sp", lambda q: q.dma_start(out=ident_f[:, :], in_=ident), writes=[b_identf])
    c.op("dve", lambda e: e.memset(ones_b[:, :], 1.0), writes=[b_ones])
    hm = sb("hm", [128, 1], F32); b_hm = c.buf("hm")
    gates4 = sb("gates4", [128, 17, 4], F32); b_gates4 = c.buf("gates4")
    dest4 = sb("dest4", [128, 17, 4], I32); b_dest4 = c.buf("dest4")
    uT = sb("uT", [128, 8, NTOK], BF16); b_uT = c.buf("uT")
    c.op("dve", lambda e: e.memset(dest4[:, :, :], 1 << 30), writes=[b_dest4])
    c.dma("sp", lambda q: q.dma_start(out=hm[:, :], in_=hmask), writes=[b_hm])

    rbx = sb("rbx", [33, 20], BF16, es2); b_rbx = c.buf("rbx")
    c.dma("pool", lambda q: q.dma_start(out=rbx[0:32, :], in_=rel_bias), writes=[b_rbx])
    c.op("dve", lambda e: e.memset(rbx[32:33, :], 1.0), reads=[b_rbx], writes=[b_rbx])
    ohs = sb("ohs", [33, 4, FVL], BF16, es2); b_ohs = c.buf("ohs")
    c.dma("pool", lambda q: q.dma_start(out=ohs[:, :, :], in_=oh), writes=[b_ohs])
    fvs = sb("fvs", [8, 4, FVL], BF16, es2); b_fvs = c.buf("fvs")
    hsets = [(0, 8), (8, 4), (12, 4), (16, 4)]
    for s, (h0, nh) in enumerate(hsets):
        MM(PS[0:nh, s, 0:FVL], rbx[:, h0:h0 + nh], ohs[:, s, :], True, True, [b_rbx, b_ohs], pb[s])
        EV(fvs[0:nh, s, :], PS[0:nh, s, 0:FVL], [pb[s]], [b_fvs])
    for s, (h0, nh) in enumerate(hsets):
        src = AP(fvs.tensor if hasattr(fvs, "tensor") else fvs, s * FVL, [[4 * FVL, nh], [0, FVR], [1, FVL]])
        c.dma("sp", lambda q: q.dma_start(out=fvd.ap()[h0:h0 + nh, :, :], in_=src),
              reads=[b_fvs], writes=[b_fvd], own=b_fvd)
    biasA = sb("biasA", [128, 3, 8, 128], BF16, es2); b_biasA = c.buf("biasA")
    biasB = sb("biasB", [128, 3, 3, 4, 128], BF16, es2); b_biasB = c.buf("biasB")

    def toep(dst, h0, nh, cc, wb):
        src = AP(fvd, h0 * FVR * FVL + cc, [[FVL - 1, 128], [FVR * FVL, nh], [1, 128]])
        c.dma("sp", lambda q: q.dma_start(out=dst, in_=src), reads=[b_fvd], writes=[wb], own=wb)

    for ty, cc in ((0, 255), (1, 127)):
        toep(biasA[:, ty, :, :], 0, 8, cc, b_biasA)
        for g in range(3):
            toep(biasB[:, g, ty, :, :], 8 + 4 * g, 4, cc, b_biasB)
    c.op("dve", lambda e: e.tensor_scalar(out=biasA[:, 2, :, :], in0=biasA[:, 0, :, :], scalar1=hm[:, 0:1],
                                          scalar2=None, op0=ALU.add), reads=[b_biasA, b_hm], writes=[b_biasA])
    for g in range(3):
        c.op("dve", lambda e: e.tensor_scalar(out=biasB[:, g, 2, :, :], in0=biasB[:, g, 0, :, :],
                                              scalar1=hm[:, 0:1], scalar2=None, op0=ALU.add),
             reads=[b_biasB, b_hm], writes=[b_biasB])

    def load_w(dst, src2d, c0, c1, wbuf, nk=8):
        v = src2d.rearrange("(k p) n -> p k n", p=128)
        c.dma("pool", lambda q: q.dma_start(out=dst, in_=v[:, :, c0:c1]), writes=[wbuf], own=wbuf)

    obT = sb("obT", [64, 4, NTOK], BF16, es2); b_obT = c.buf("obT")

    xsT = sb("xsT", [128, 8, NS], BF16, es2); b_xsT = c.buf("xsT")
    c.dma("pool", lambda q: q.dma_start(out=xsT[:, :, :], in_=xst.rearrange("(k p) n -> p k n", p=128)),
          writes=[b_xsT])

    with ExitStack() as ph:
        wB = sb("wB", [128, 8, 1280], BF16, ph); b_wB = c.buf("wB")
        load_w(wB[:, :, :], w_in, 768, 2048, b_wB)
        kvo = [sb("kvoB%d" % i, [128, 512], F32, ph) for i in range(2)]
        b_kvo = c.bufs(2, "kvoB")
        qsT = sb("qsTB", [64, 12, NS], BF16, ph); b_qsT = c.buf("qsTB")
        ksT = sb("ksTB", [64, 4, NS], BF16, ph); b_ksT = c.buf("ksTB")
        ph1 = ExitStack()
        xp = [sb("xpB%d" % i, [128, 8, 512], BF16, ph1) for i in range(4)]
        b_xp = c.bufs(4, "xpB")
        pT = [sb("pTB%d" % i, [128, 512], BF16, ph1) for i in range(2)]
        b_pT = c.bufs(2, "pTB")
        KT = sb("KTB", [64, 2, 32 * 128], BF16, ph1); b_KT = c.buf("KTB")
        VV = sb("VVB", [128, 32, 128], BF16, ph1); b_VV = c.buf("VVB")
        QT = sb("QTB", [64, 2, NT], BF16, ph1); b_QT = c.buf("QTB")
        b_KT1 = c.buf("KTB1"); b_QT1 = c.buf("QTB1")
        stgK = [sb("stgK%d" % i, [128, 512], BF16, ph1) for i in range(2)]; b_stgK = c.bufs(2, "stgK")
        stgQ = [sb("stgQ%d" % i, [128, 512], BF16, ph1) for i in range(2)]; b_stgQ = c.bufs(2, "stgQ")
        acc = sb("accB", [64, 4, NT], F32, ph1); b_acc = c.buf("accB")
        npiece = 0
        for hp in range(2):
            for g in range(3):
                nH = NH_G[g]
                nblk = nH + 16
                xv = xt[g].rearrange("(k p) n -> p k n", p=128)
                for p0 in range(0, nblk, 4):
                    nb_ = min(4, nblk - p0)
                    ntk = nb_ * 128
                    xb = npiece % 4
                    npiece += 1
                    c.dma("pool", lambda q: q.dma_start(out=xp[xb][:, :, 0:ntk], in_=xv[:, :, p0 * 128:p0 * 128 + ntk]),
                          writes=[b_xp[xb]])
                    sgi = npiece % 2
                    for k in range(8):
                        MM(PS[:, 0, 0:ntk], wB[:, k, 768 + 128 * hp:768 + 128 * hp + 128], xp[xb][:, k, 0:ntk],
                           k == 0, k == 7, [b_wB, b_xp[xb]], pb[0])
                    EV(KT[:, 0, p0 * 128:p0 * 128 + ntk], PS[0:64, 0, 0:ntk], [pb[0]], [b_KT])
                    EV(stgK[sgi][64:128, 0:ntk], PS[64:128, 0, 0:ntk], [pb[0]], [b_stgK[sgi]])
                    c.dma("sp", lambda q: q.dma_start(out=KT[:, 1, p0 * 128:p0 * 128 + ntk], in_=stgK[sgi][64:128, 0:ntk]),
                          reads=[b_stgK[sgi]], writes=[b_KT1], own=b_stgK[sgi])
                    o0 = max(p0, nH)
                    if o0 < p0 + nb_:
                        lo = (o0 - p0) * 128
                        nq = ntk - lo
                        qc = slice((o0 - nH) * 128, (o0 - nH) * 128 + nq)
                        for k in range(8):
                            MM(PS[:, 1, 0:nq], wB[:, k, 256 * g + 128 * hp:256 * g + 128 * hp + 128],
                               xp[xb][:, k, lo:ntk], k == 0, k == 7, [b_wB, b_xp[xb]], pb[1])
                        EV(QT[:, 0, qc], PS[0:64, 1, 0:nq], [pb[1]], [b_QT], scale=0.125)
                        EV(stgQ[sgi][64:128, 0:nq], PS[64:128, 1, 0:nq], [pb[1]], [b_stgQ[sgi]], scale=0.125)
                        c.dma("sp", lambda q: q.dma_start(out=QT[:, 1, qc], in_=stgQ[sgi][64:128, 0:nq]),
                              reads=[b_stgQ[sgi]], writes=[b_QT1], own=b_stgQ[sgi])
                    for j in range(nb_):
                        blk = p0 + j
                        for k in range(8):
                            MM(PS[:, 4, j * 128:j * 128 + 128], xp[xb][:, k, j * 128:j * 128 + 128],
                               wB[:, k, 1024 + 128 * hp:1024 + 128 * hp + 128], k == 0, k == 7,
                               [b_wB, b_xp[xb]], pb[4])
                    EV(VV[:, p0:p0 + nb_, :], PS[:, 4, 0:ntk].rearrange("p (j n) -> p j n", n=128), [pb[4]], [b_VV])
                    if g == 0 and hp == 0:
                        for j in range(nb_):
                            blk = p0 + j
                            if blk < nH:
                                continue
                            ko = blk % 2
                            for k in range(8):
                                MM(PS[:, 5, :], xp[xb][:, k, j * 128:j * 128 + 128], wB[:, k, 768:1280],
                                   k == 0, k == 7, [b_wB, b_xp[xb]], pb[5])
                            EV(kvo[ko][:, :], PS[:, 5, :], [pb[5]], [b_kvo[ko]])
                            t0 = (blk - nH) * 128
                            c.dma("sp", lambda q: q.dma_start(out=bk_o[t0:t0 + 128, :], in_=kvo[ko][:, 0:256]),
                                  reads=[b_kvo[ko]], own=b_kvo[ko], final=True)
                            c.dma("sp", lambda q: q.dma_start(out=bv_o[t0:t0 + 128, :], in_=kvo[ko][:, 256:512]),
                                  reads=[b_kvo[ko]], own=b_kvo[ko], final=True)
                for j in range(16):
                    cur = nH + j
                    if g == 0:
                        prev, halo = cur - 1, (j == 0)
                    elif g == 1:
                        r, m = j // 4, j % 4
                        prev, halo = (r, True) if m == 0 else (cur - 1, False)
                    else:
                        prev, halo = j, True
                    sbk_ = 6 + (j % 2)
                    pi = j % 2
                    for ty, kb_ in ((0, prev), (1, cur)):
                        bty = 2 if (ty == 0 and halo) else ty
                        for hh in range(2):
                            MM(PS[:, sbk_, ty * 256 + hh * 128:ty * 256 + hh * 128 + 128],
                               KT[:, hh, kb_ * 128:kb_ * 128 + 128], QT[:, hh, j * 128:j * 128 + 128],
                               True, False, [b_KT1, b_QT1] if hh else [b_KT, b_QT], pb[sbk_])
                        MM(PS[:, sbk_, ty * 256:ty * 256 + 256], ident_b[:, :],
                           biasB[:, g, bty, 2 * hp:2 * hp + 2, :], False, True, [b_ident, b_biasB], pb[sbk_])
                    c.op("act", lambda e: e.activation(out=pT[pi][:, :], in_=PS[:, sbk_, :], func=AF.Exp),
                         reads=[pb[sbk_]], writes=[b_pT[pi]])
                    ob_ = 2 + (j % 2)
                    for hh in range(2):
                        MM(PS[0:64, ob_, hh * 128:hh * 128 + 128], VV[:, prev, hh * 64:hh * 64 + 64],
                           pT[pi][:, hh * 128:hh * 128 + 128], True, False, [b_VV, b_pT[pi]], pb[ob_])
                        MM(PS[0:64, ob_, hh * 128:hh * 128 + 128], VV[:, cur, hh * 64:hh * 64 + 64],
                           pT[pi][:, 256 + hh * 128:256 + hh * 128 + 128], False, True, [b_VV, b_pT[pi]], pb[ob_])
                    MM(PS[0:64, ob_, 256:512], ones_b[:, 0:64], pT[pi][:, 0:256], True, False, [b_ones, b_pT[pi]], pb[ob_])
                    MM(PS[0:64, ob_, 256:512], ones_b[:, 0:64], pT[pi][:, 256:512], False, True, [b_ones, b_pT[pi]], pb[ob_])
                    if g == 0:
                        off, st = j * 128, 1
                    elif g == 1:
                        off, st = 512 * (j % 4) + (j // 4), 4
                    else:
                        off, st = j, 16
                    av_ = AP(acc, off, [[4 * NT, 64], [NT, 4], [st, 128]])
                    src = PS[0:64, ob_, :].rearrange("p (a n) -> p a n", n=128)
                    if g == 0:
                        c.op("dve", lambda e: e.tensor_copy(out=av_, in_=src), reads=[pb[ob_]], writes=[b_acc])
                    else:
                        c.op("dve", lambda e: e.tensor_tensor(out=av_, in0=src, in1=av_, op=ALU.add),
                             reads=[pb[ob_], b_acc], writes=[b_acc])
            c.op("dve", lambda e: e.reciprocal(out=acc[:, 2:4, :], in_=acc[:, 2:4, :]), reads=[b_acc], writes=[b_acc])
            c.op("dve", lambda e: e.tensor_tensor(out=obT[:, 2 * hp:2 * hp + 2, 0:NT], in0=acc[:, 0:2, :],
                                                  in1=acc[:, 2:4, :], op=ALU.mult),
                 reads=[b_acc], writes=[b_obT])
        c.barrier()
        ph1.close()
        for hd in range(16):
            col = (64 * hd) if hd < 12 else (768 + 64 * (hd - 12))
            bk = hd % 2
            for k in range(8):
                MM(PS[0:64, bk, 0:NS], wB[:, k, col:col + 64], xsT[:, k, :], k == 0, k == 7, [b_wB, b_xsT], pb[bk])
            if hd < 12:
                EV(qsT[:, hd, :], PS[0:64, bk, 0:NS], [pb[bk]], [b_qsT], scale=0.125)
            else:
                EV(ksT[:, hd - 12, :], PS[0:64, bk, 0:NS], [pb[bk]], [b_ksT])
        for k in range(8):
            MM(PS[0:NS, 5, :], xsT[:, k, :], wB[:, k, 768:1280], k == 0, k == 7, [b_wB, b_xsT], pb[5])
        EV(kvo[0][0:NS, :], PS[0:NS, 5, :], [pb[5]], [b_kvo[0]])
        c.dma("sp", lambda q: q.dma_start(out=sbk_o[:, :], in_=kvo[0][0:NS, 0:256]), reads=[b_kvo[0]], own=b_kvo[0], final=True)
        c.dma("sp", lambda q: q.dma_start(out=sbv_o[:, :], in_=kvo[0][0:NS, 256:512]), reads=[b_kvo[0]], own=b_kvo[0], final=True)
        vtmp = sb("vtmpB", [NS, 256], BF16, ph); b_vtmp = c.buf("vtmpB")
        EV(vtmp[:, :], kvo[0][0:NS, 256:512], [b_kvo[0]], [b_vtmp], eng="dve")
        c.dma("sp", lambda q: q.dma_start(out=vnd.ap()[:, 128:384], in_=vtmp[:, :]), reads=[b_vtmp], writes=[b_vnd], own=b_vtmp)
        vnB = sb("vnB", [4, NB, 256], BF16, ph); b_vnB = c.buf("vnB")
        c.dma("sp", lambda q: q.dma_start(out=vnB[:, :, :], in_=vnd.ap()[:, 128:384].rearrange("(b i) n -> i b n", i=4)),
              reads=[b_vnd], writes=[b_vnB])
        bbc = sb("bbc", [128, 3, 4, 4], F32, ph); b_bbc = c.buf("bbc")
        bbn = sb("bbn", [4, 3, 4, 4], F32, ph); b_bbn = c.buf("bbn")
        offd = sb("offd", [4, 4], F32, ph); b_offd = c.buf("offd")
        c.op("dve", lambda e: e.tensor_scalar(out=offd[:, :], in0=ident_f[0:4, 0:4], scalar1=-1.0, scalar2=-NEG,
                                              op0=ALU.add, op1=ALU.mult), reads=[b_identf], writes=[b_offd])
        c.op("dve", lambda e: e.tensor_copy(out=bbc[:, 0, :, :], in_=biasB[:, 0, 0, :, 0:4]), reads=[b_biasB], writes=[b_bbc])
        c.op("dve", lambda e: e.tensor_copy(out=bbn[:, 0, :, :], in_=biasB[0:4, 0, 1, :, 0:4]), reads=[b_biasB], writes=[b_bbn])
        for g in (1, 2):
            c.op("dve", lambda e: e.tensor_copy(out=bbc[:, g, :, :], in_=biasB[:, g, 0, :, 0:1].to_broadcast([128, 4, 4])),
                 reads=[b_biasB], writes=[b_bbc])
            c.op("dve", lambda e: e.tensor_tensor(out=bbn[:, g, :, :], in0=biasB[0:4, g, 1, :, 0:4],
                                                  in1=offd[:, :].unsqueeze(1).to_broadcast([4, 4, 4]), op=ALU.add),
                 reads=[b_biasB, b_offd], writes=[b_bbn])
        kcb = [sb("kcb%d" % i, [64, 9, 4, 128], BF16, ph) for i in range(2)]; b_kcb = c.bufs(2, "kcb")
        vcb = [sb("vcb%d" % i, [128, 9, 4, 64], BF16, ph) for i in range(2)]; b_vcb = c.bufs(2, "vcb")
        sc_f = sb("scfB", [128, 48], F32, ph); b_scf = c.buf("scfB")
        sn_f = sb("snfB", [4, 48], F32, ph); b_snf = c.buf("snfB")
        pc_b = sb("pcB", [128, 48], BF16, ph); b_pc = c.buf("pcB")
        pn_b = sb("pnB", [4, 48], BF16, ph); b_pn = c.buf("pnB")
        pns = sb("pnsB", [4, 16], BF16, ph); b_pns = c.buf("pnsB")
        rcs = sb("rcsB", [64, 16], F32, ph); b_rcs = c.buf("rcsB")
        for b in range(NB):
            bb = b % 2
            c.dma("pool", lambda q: q.dma_start(out=kcb[bb][:, :, :, :].rearrange("p s h n -> p (s h n)"), in_=cbkT[b]),
                  writes=[b_kcb[bb]])
            c.dma("pool", lambda q: q.dma_start(out=vcb[bb][:, :, :, :].rearrange("p s h n -> p (s h n)"), in_=cbv[b]),
                  writes=[b_vcb[bb]])
            tk = slice(4 * b, 4 * b + 4)
            for h in range(4):
                MM(PS[:, 0, h * 4:h * 4 + 4], kcb[bb][:, 0, h, :], qsT[:, h, tk], True, True, [b_kcb[bb], b_qsT], pb[0])
                for g in (1, 2):
                    for i in range(4):
                        cix = g * 16 + h * 4 + i
                        MM(PS[:, 0, cix:cix + 1], kcb[bb][:, 1 + 4 * (g - 1) + i, h, :],
                           qsT[:, 4 * g + h, 4 * b + i:4 * b + i + 1], True, True, [b_kcb[bb], b_qsT], pb[0])
                MM(PS[0:4, 1, :48].rearrange("p (g h i) -> p g h i", g=3, h=4)[:, :, h, :], ksT[:, h, tk],
                   AP(qsT, h * NS + 4 * b, [[12 * NS, 64], [4 * NS, 3], [1, 4]]), True, True, [b_ksT, b_qsT], pb[1])
            c.op("dve", lambda e: e.tensor_tensor(out=sc_f[:, :], in0=PS[:, 0, 0:48],
                                                  in1=bbc[:, :, :, :].rearrange("p g h i -> p (g h i)"), op=ALU.add),
                 reads=[pb[0], b_bbc], writes=[b_scf])
            c.op("dve", lambda e: e.tensor_tensor(out=sn_f[:, :], in0=PS[0:4, 1, 0:48],
                                                  in1=bbn[:, :, :, :].rearrange("p g h i -> p (g h i)"), op=ALU.add),
                 reads=[pb[1], b_bbn], writes=[b_snf])
            c.op("act", lambda e: e.activation(out=pc_b[:, :], in_=sc_f[:, :], func=AF.Exp), reads=[b_scf], writes=[b_pc])
            c.op("act", lambda e: e.activation(out=pn_b[:, :], in_=sn_f[:, :], func=AF.Exp), reads=[b_snf], writes=[b_pn])
            c.op("dve", lambda e: e.tensor_tensor(out=pns[:, :], in0=pn_b[:, 0:16], in1=pn_b[:, 16:32], op=ALU.add),
                 reads=[b_pn], writes=[b_pns])
            c.op("dve", lambda e: e.tensor_tensor(out=pns[:, :], in0=pns[:, :], in1=pn_b[:, 32:48], op=ALU.add),
                 reads=[b_pn, b_pns], writes=[b_pns])
            for h in range(4):
                MM(PS[0:64, 2, h * 4:h * 4 + 4], vcb[bb][:, 0, h, :], pc_b[:, h * 4:h * 4 + 4], True, False,
                   [b_vcb[bb], b_pc], pb[2])
                for g in (1, 2):
                    for i in range(4):
                        cix = g * 16 + h * 4 + i
                        MM(PS[0:64, 2, h * 4 + i:h * 4 + i + 1], vcb[bb][:, 1 + 4 * (g - 1) + i, h, :],
                           pc_b[:, cix:cix + 1], False, False, [b_vcb[bb], b_pc], pb[2])
                MM(PS[0:64, 2, h * 4:h * 4 + 4], vnB[:, b, 64 * h:64 * h + 64], pns[:, h * 4:h * 4 + 4], False, True,
                   [b_vnB, b_pns], pb[2])
            for g in range(3):
                MM(PS[0:64, 3, 0:16], ones_b[:, 0:64], pc_b[:, g * 16:g * 16 + 16], g == 0, False, [b_ones, b_pc], pb[3])
            MM(PS[0:64, 3, 0:16], ones_b[0:4, 0:64], pns[:, :], False, True, [b_ones, b_pns], pb[3])
            c.op("dve", lambda e: e.reciprocal(out=rcs[:, :], in_=PS[0:64, 3, 0:16]), reads=[pb[3]], writes=[b_rcs])
            c.op("dve", lambda e: e.tensor_tensor(out=obT[:, :, NT + 4 * b:NT + 4 * b + 4],
                                                  in0=PS[0:64, 2, 0:16].rearrange("p (h i) -> p h i", i=4),
                                                  in1=rcs[:, :].rearrange("p (h i) -> p h i", i=4), op=ALU.mult),
                 reads=[pb[2], b_rcs], writes=[b_obT])
        c.barrier()
        if stop == "B":
            c.finish("sp")
            return nc

    oaT = sb("oaT", [64, 8, NTOK], BF16, es2); b_oaT = c.buf("oaT")
    xT = sb("xT", [128, 8, 17 * 128], BF16, es2); b_xT = c.buf("xT")
    c.dma("pool", lambda q: q.dma_start(out=xT[:, :, :], in_=xt[0].rearrange("(k p) n -> p k n", p=128)), writes=[b_xT])
    es8 = sb("es8", [64, 8], F32, es2); b_es8 = c.buf("es8")
    c.dma("sp", lambda q: q.dma_start(out=es8[:, :], in_=AP(sinks.tensor, 0, [[0, 64], [1, 8]])), writes=[b_es8])
    c.op("act", lambda e: e.activation(out=es8[:, :], in_=es8[:, :], func=AF.Exp), reads=[b_es8], writes=[b_es8])
    with ExitStack() as ph:
        wA = sb("wA", [128, 8, 768], BF16, ph); b_wA = c.buf("wA")
        load_w(wA[:, :, :], w_in, 0, 768, b_wA)
        dtm = sb("dtmA", [64, 512], F32, ph); b_dtm = c.buf("dtmA")
        kvoA = sb("kvoA", [128, 256], F32, ph); b_kvoA = c.buf("kvoA")
        ph1 = ExitStack()
        KTA = sb("KTA", [64, 2, 17 * 128], BF16, ph1); b_KTA = c.buf("KTA")
        VA = sb("VA", [128, 17, 128], BF16, ph1); b_VA = c.buf("VA")
        QTA = sb("QTA", [64, 8, NT], BF16, ph1); b_QTA = c.buf("QTA")
        pTA = [sb("pTA%d" % i, [128, 2, 512], BF16, ph1) for i in range(2)]; b_pTA = c.bufs(2, "pTA")
        for p0 in range(0, 17, 4):
            nb_ = min(4, 17 - p0)
            ntk = nb_ * 128
            cs = slice(p0 * 128, p0 * 128 + ntk)
            for g in range(2):
                for k in range(8):
                    MM(PS[0:64, g, 0:ntk], wA[:, k, 512 + 64 * g:512 + 64 * g + 64], xT[:, k, cs], k == 0, k == 7,
                       [b_wA, b_xT], pb[g])
                EV(KTA[:, g, cs], PS[0:64, g, 0:ntk], [pb[g]], [b_KTA])
            o0 = max(p0, 1)
            lo = (o0 - p0) * 128
            nq = ntk - lo
            if nq > 0:
                for h in range(8):
                    bq = 2 + (h % 2)
                    for k in range(8):
                        MM(PS[0:64, bq, 0:nq], wA[:, k, 64 * h:64 * h + 64], xT[:, k, o0 * 128:o0 * 128 + nq], k == 0, k == 7,
                           [b_wA, b_xT], pb[bq])
                    EV(QTA[:, h, (o0 - 1) * 128:(o0 - 1) * 128 + nq], PS[0:64, bq, 0:nq], [pb[bq]], [b_QTA], scale=0.125)
            for j in range(nb_):
                for k in range(8):
                    MM(PS[:, 4, j * 128:j * 128 + 128], xT[:, k, (p0 + j) * 128:(p0 + j) * 128 + 128], wA[:, k, 640:768],
                       k == 0, k == 7, [b_wA, b_xT], pb[4])
            EV(VA[:, p0:p0 + nb_, :], PS[:, 4, 0:ntk].rearrange("p (j n) -> p j n", n=128), [pb[4]], [b_VA])
        for k in range(8):
            MM(PS[:, 5, 0:256], xT[:, k, 16 * 128:17 * 128], wA[:, k, 512:768], k == 0, k == 7, [b_wA, b_xT], pb[5])
        EV(kvoA[:, :], PS[:, 5, 0:256], [pb[5]], [b_kvoA])
        c.dma("sp", lambda q: q.dma_start(out=ak_o[:, :], in_=kvoA[:, 0:128]), reads=[b_kvoA], own=b_kvoA, final=True)
        c.dma("sp", lambda q: q.dma_start(out=av_o[:, :], in_=kvoA[:, 128:256]), reads=[b_kvoA], own=b_kvoA, final=True)
        for gg in range(2):
            for j in range(16):
                pi = j % 2
                for ty, kb_ in ((0, j), (1, j + 1)):
                    bty = 2 if (ty == 0 and j == 0) else ty
                    sbk_ = 6 + ty
                    MM(PS[:, sbk_, :], KTA[:, gg, kb_ * 128:kb_ * 128 + 128], QTA[:, 4 * gg:4 * gg + 4, j * 128:j * 128 + 128],
                       True, False, [b_KTA, b_QTA], pb[sbk_])
                    MM(PS[:, sbk_, :], ident_b[:, :], biasA[:, bty, 4 * gg:4 * gg + 4, :], False, True,
                       [b_ident, b_biasA], pb[sbk_])
                    c.op("act", lambda e: e.activation(out=pTA[pi][:, ty, :], in_=PS[:, sbk_, :], func=AF.Exp),
                         reads=[pb[sbk_]], writes=[b_pTA[pi]])
                ob_, db_ = 0 + (j % 2), 2 + (j % 2)
                MM(PS[0:64, ob_, :], VA[:, j, 64 * gg:64 * gg + 64], pTA[pi][:, 0, :], True, False, [b_VA, b_pTA[pi]], pb[ob_])
                MM(PS[0:64, ob_, :], VA[:, j + 1, 64 * gg:64 * gg + 64], pTA[pi][:, 1, :], False, True, [b_VA, b_pTA[pi]], pb[ob_])
                MM(PS[0:64, db_, :], ones_b[:, 0:64], pTA[pi][:, 0, :], True, False, [b_ones, b_pTA[pi]], pb[db_])
                MM(PS[0:64, db_, :], ones_b[:, 0:64], pTA[pi][:, 1, :], False, True, [b_ones, b_pTA[pi]], pb[db_])
                c.op("dve", lambda e: e.tensor_tensor(out=dtm[:, :].rearrange("p (h n) -> p h n", n=128),
                                                      in0=PS[0:64, db_, :].rearrange("p (h n) -> p h n", n=128),
                                                      in1=es8[:, 4 * gg:4 * gg + 4].unsqueeze(2).to_broadcast([64, 4, 128]),
                                                      op=ALU.add), reads=[pb[db_], b_es8], writes=[b_dtm])
                c.op("dve", lambda e: e.reciprocal(out=dtm[:, :], in_=dtm[:, :]), reads=[b_dtm], writes=[b_dtm])
                c.op("dve", lambda e: e.tensor_tensor(out=oaT[:, 4 * gg:4 * gg + 4, j * 128:j * 128 + 128],
                                                      in0=PS[0:64, ob_, :].rearrange("p (h n) -> p h n", n=128),
                                                      in1=dtm[:, :].rearrange("p (h n) -> p h n", n=128), op=ALU.mult),
                     reads=[pb[ob_], b_dtm], writes=[b_oaT])
        c.barrier()
        ph1.close()
        qaS = sb("qaS", [64, 8, NS], BF16, ph); b_qaS = c.buf("qaS")
        kaS = sb("kaS", [64, 2, NS], BF16, ph); b_kaS = c.buf("kaS")
        for hd in range(10):
            col = 64 * hd
            bk = hd % 2
            for k in range(8):
                MM(PS[0:64, bk, 0:NS], wA[:, k, col:col + 64], xsT[:, k, :], k == 0, k == 7, [b_wA, b_xsT], pb[bk])
            if hd < 8:
                EV(qaS[:, hd, :], PS[0:64, bk, 0:NS], [pb[bk]], [b_qaS], scale=0.125)
            else:
                EV(kaS[:, hd - 8, :], PS[0:64, bk, 0:NS], [pb[bk]], [b_kaS])
        for k in range(8):
            MM(PS[0:NS, 5, 0:256], xsT[:, k, :], wA[:, k, 512:768], k == 0, k == 7, [b_wA, b_xsT], pb[5])
        EV(kvoA[0:NS, :], PS[0:NS, 5, 0:256], [pb[5]], [b_kvoA])
        c.dma("sp", lambda q: q.dma_start(out=sak_o[:, :], in_=kvoA[0:NS, 0:128]), reads=[b_kvoA], own=b_kvoA, final=True)
        c.dma("sp", lambda q: q.dma_start(out=sav_o[:, :], in_=kvoA[0:NS, 128:256]), reads=[b_kvoA], own=b_kvoA, final=True)
        vtA = sb("vtA", [NS, 128], BF16, ph); b_vtA = c.buf("vtA")
        EV(vtA[:, :], kvoA[0:NS, 128:256], [b_kvoA], [b_vtA], eng="dve")
        c.dma("sp", lambda q: q.dma_start(out=vnd.ap()[:, 0:128], in_=vtA[:, :]), reads=[b_vtA], writes=[b_vnd], own=b_vtA)
        vnA = sb("vnA", [4, NB, 128], BF16, ph); b_vnA = c.buf("vnA")
        c.dma("sp", lambda q: q.dma_start(out=vnA[:, :, :], in_=vnd.ap()[:, 0:128].rearrange("(b i) n -> i b n", i=4)),
              reads=[b_vnd], writes=[b_vnA])
        KcA = sb("KcA", [64, NB, 2, 128], BF16, ph); b_KcA = c.buf("KcA")
        VcA = sb("VcA", [128, NB, 128], BF16, ph); b_VcA = c.buf("VcA")
        c.dma("pool", lambda q: q.dma_start(out=KcA[:, :, :, :], in_=cakT), writes=[b_KcA])
        c.dma("pool", lambda q: q.dma_start(out=VcA[:, :, :], in_=cav), writes=[b_VcA])
        scA = sb("scA", [128, 512], F32, ph); b_scA = c.buf("scA")
        snA = sb("snA", [4, 512], F32, ph); b_snA = c.buf("snA")
        pcA = sb("pcA", [128, 512], BF16, ph); b_pcA = c.buf("pcA")
        pnA = sb("pnA", [4, 512], BF16, ph); b_pnA = c.buf("pnA")
        for b in range(NB):
            for g in range(2):
                cs = slice((2 * b + g) * 16, (2 * b + g) * 16 + 16)
                qv = AP(qaS, 4 * g * NS + 4 * b, [[8 * NS, 64], [NS, 4], [1, 4]])
                MM(PS[:, 6, cs], KcA[:, b, g, :], qv, True, True, [b_KcA, b_qaS], pb[6])
                MM(PS[0:4, 7, cs], kaS[:, g, 4 * b:4 * b + 4], qv, True, True, [b_kaS, b_qaS], pb[7])
        c.op("dve", lambda e: e.tensor_tensor(out=scA[:, :].rearrange("p (b h i) -> p b h i", b=NB, h=8),
                                              in0=PS[:, 6, :].rearrange("p (b h i) -> p b h i", b=NB, h=8),
                                              in1=AP(biasA, 0, [[3072, 128], [0, NB], [128, 8], [1, 4]]), op=ALU.add),
             reads=[pb[6], b_biasA], writes=[b_scA])
        c.op("dve", lambda e: e.tensor_tensor(out=snA[:, :].rearrange("p (b h i) -> p b h i", b=NB, h=8),
                                              in0=PS[0:4, 7, :].rearrange("p (b h i) -> p b h i", b=NB, h=8),
                                              in1=AP(biasA, 1024, [[3072, 4], [0, NB], [128, 8], [1, 4]]), op=ALU.add),
             reads=[pb[7], b_biasA], writes=[b_snA])
        c.op("act", lambda e: e.activation(out=pcA[:, :], in_=scA[:, :], func=AF.Exp), reads=[b_scA], writes=[b_pcA])
        c.op("act", lambda e: e.activation(out=pnA[:, :], in_=snA[:, :], func=AF.Exp), reads=[b_snA], writes=[b_pnA])
        for b in range(NB):
            for g in range(2):
                cs = slice((2 * b + g) * 16, (2 * b + g) * 16 + 16)
                MM(PS[0:64, 0, cs], VcA[:, b, 64 * g:64 * g + 64], pcA[:, cs], True, False, [b_VcA, b_pcA], pb[0])
                MM(PS[0:64, 0, cs], vnA[:, b, 64 * g:64 * g + 64], pnA[:, cs], False, True, [b_vnA, b_pnA], pb[0])
        MM(PS[0:64, 1, :], ones_b[:, 0:64], pcA[:, :], True, False, [b_ones, b_pcA], pb[1])
        MM(PS[0:64, 1, :], ones_b[0:4, 0:64], pnA[:, :], False, True, [b_ones, b_pnA], pb[1])
        c.op("dve", lambda e: e.tensor_tensor(out=dtm[:, :].rearrange("p (b h i) -> p b h i", b=NB, h=8),
                                              in0=PS[0:64, 1, :].rearrange("p (b h i) -> p b h i", b=NB, h=8),
                                              in1=AP(es8, 0, [[8, 64], [0, NB], [1, 8], [0, 4]]), op=ALU.add),
             reads=[pb[1], b_es8], writes=[b_dtm])
        c.op("dve", lambda e: e.reciprocal(out=dtm[:, :], in_=dtm[:, :]), reads=[b_dtm], writes=[b_dtm])
        c.op("dve", lambda e: e.tensor_tensor(out=AP(oaT, NT, [[8 * NTOK, 64], [4, NB], [NTOK, 8], [1, 4]]),
                                              in0=PS[0:64, 0, :].rearrange("p (b h i) -> p b h i", b=NB, h=8),
                                              in1=dtm[:, :].rearrange("p (b h i) -> p b h i", b=NB, h=8), op=ALU.mult),
             reads=[pb[0], b_dtm], writes=[b_oaT])
        c.barrier()
        if stop == "A":
            c.finish("sp")
            return nc

    omT = sb("omT", [128, 4, NTOK], BF16, es2); b_omT = c.buf("omT")
    with ExitStack() as ph:
        wM = sb("wM", [128, 8, 512], BF16, ph); b_wM = c.buf("wM")
        load_w(wM[:, :, :], w_in, 2048, 2560, b_wM)
        wmk = sb("wmk", [128, 8, 1024], BF16, ph); b_wmk = c.buf("wmk")
        load_w(wmk[:, :, :], w_mem_kv, 0, 1024, b_wmk)
        mTs = sb("mTs", [128, 8, 256], BF16, ph); b_mTs = c.buf("mTs")
        c.dma("pool", lambda q: q.dma_start(out=mTs[:, :, :], in_=memT.rearrange("(k p) n -> p k n", p=128)), writes=[b_mTs])
        KMT = sb("KMT", [128, 4, 256], BF16, ph); b_KMT = c.buf("KMT")
        VM = sb("VM", [128, 2, 512], BF16, ph); b_VM = c.buf("VM")
        mo = [sb("moM0", [128, 512], F32, ph)] * 2; b_mo = [c.buf("moM")] * 2
        for h in range(4):
            for k in range(8):
                MM(PS[:, h % 2, 0:256], wmk[:, k, 128 * h:128 * h + 128], mTs[:, k, :], k == 0, k == 7, [b_wmk, b_mTs], pb[h % 2])
            EV(KMT[:, h, :], PS[:, h % 2, 0:256], [pb[h % 2]], [b_KMT])
        for t in range(2):
            for half, dst in ((0, mk_o), (1, mv_o)):
                bk = 2 + half
                for k in range(8):
                    MM(PS[:, bk, :], mTs[:, k, 128 * t:128 * t + 128], wmk[:, k, 512 * half:512 * half + 512], k == 0, k == 7,
                       [b_wmk, b_mTs], pb[bk])
                mi = (2 * t + half) % 2
                EV(mo[mi][:, :], PS[:, bk, :], [pb[bk]], [b_mo[mi]])
                if half == 1:
                    EV(VM[:, t, :], mo[mi][:, :], [b_mo[mi]], [b_VM], eng="dve")
                c.dma("sp", lambda q: q.dma_start(out=dst[128 * t:128 * t + 128, :], in_=mo[mi][:, :]),
                      reads=[b_mo[mi]], own=b_mo[mi], final=True)
        QMT = [sb("QMT%d" % i, [128, 512], BF16, ph) for i in range(2)]; b_QMT = c.bufs(2, "QMT")
        pM = [sb("pM%d" % i, [128, 2, 512], BF16, ph) for i in range(2)]; b_pM = c.bufs(2, "pM")
        dtM = sb("dtM", [128, 512], F32, ph); b_dtM = c.buf("dtM")
        MSC = float(128.0 ** -0.5)
        it = 0
        for h in range(4):
            for tg in range(4):
                qi = it % 2
                it += 1
                cs = slice(128 + 512 * tg, 128 + 512 * tg + 512)
                for k in range(8):
                    MM(PS[:, qi, :], wM[:, k, 128 * h:128 * h + 128], xT[:, k, cs], k == 0, k == 7, [b_wM, b_xT], pb[qi])
                EV(QMT[qi][:, :], PS[:, qi, :], [pb[qi]], [b_QMT[qi]], scale=MSC)
                for t in range(2):
                    MM(PS[:, 4 + t, :], KMT[:, h, 128 * t:128 * t + 128], QMT[qi][:, :], True, True, [b_KMT, b_QMT[qi]], pb[4 + t])
                    c.op("act", lambda e: e.activation(out=pM[qi][:, t, :], in_=PS[:, 4 + t, :], func=AF.Exp),
                         reads=[pb[4 + t]], writes=[b_pM[qi]])
                for t in range(2):
                    MM(PS[:, 6, :], VM[:, t, 128 * h:128 * h + 128], pM[qi][:, t, :], t == 0, t == 1, [b_VM, b_pM[qi]], pb[6])
                for t in range(2):
                    MM(PS[:, 7, :], ones_b[:, :], pM[qi][:, t, :], t == 0, t == 1, [b_ones, b_pM[qi]], pb[7])
                c.op("dve", lambda e: e.reciprocal(out=dtM[:, :], in_=PS[:, 7, :]), reads=[pb[7]], writes=[b_dtM])
                c.op("dve", lambda e: e.tensor_tensor(out=omT[:, h, 512 * tg:512 * tg + 512], in0=PS[:, 6, :], in1=dtM[:, :],
                                                      op=ALU.mult), reads=[pb[6], b_dtM], writes=[b_omT])
        qmS = sb("qmS", [128, 4, NS], BF16, ph); b_qmS = c.buf("qmS")
        for h in range(4):
            for k in range(8):
                MM(PS[:, h % 2, 0:NS], wM[:, k, 128 * h:128 * h + 128], xsT[:, k, :], k == 0, k == 7, [b_wM, b_xsT], pb[h % 2])
            EV(qmS[:, h, :], PS[:, h % 2, 0:NS], [pb[h % 2]], [b_qmS], scale=MSC)
        KcM = [sb("KcM%d" % i, [128, 4, 256], BF16, ph) for i in range(2)]; b_KcM = c.bufs(2, "KcM")
        VcM = [sb("VcM%d" % i, [128, 2, 512], BF16, ph) for i in range(2)]; b_VcM = c.bufs(2, "VcM")
        p32 = [sb("p32_%d" % i, [128, 32], BF16, ph) for i in range(2)]; b_p32 = c.bufs(2, "p32")
        for b in range(NB):
            bb = b % 2
            c.dma("pool", lambda q: q.dma_start(out=KcM[bb][:, :, :].rearrange("p h n -> p (h n)"), in_=cmkT[b]), writes=[b_KcM[bb]])
            c.dma("pool", lambda q: q.dma_start(out=VcM[bb][:, :, :].rearrange("p t n -> p (t n)"), in_=cmv[b]), writes=[b_VcM[bb]])
            sbk_ = 4 + bb
            for t in range(2):
                for h in range(4):
                    cix = t * 16 + h * 4
                    MM(PS[:, sbk_, cix:cix + 4], KcM[bb][:, h, 128 * t:128 * t + 128], qmS[:, h, 4 * b:4 * b + 4], True, True,
                       [b_KcM[bb], b_qmS], pb[sbk_])
            c.op("act", lambda e: e.activation(out=p32[bb][:, :], in_=PS[:, sbk_, 0:32], func=AF.Exp), reads=[pb[sbk_]], writes=[b_p32[bb]])
            for h in range(4):
                for t in range(2):
                    MM(PS[:, 6, b * 16 + h * 4:b * 16 + h * 4 + 4], VcM[bb][:, t, 128 * h:128 * h + 128],
                       p32[bb][:, t * 16 + h * 4:t * 16 + h * 4 + 4], t == 0, t == 1, [b_VcM[bb], b_p32[bb]], pb[6])
            for t in range(2):
                MM(PS[:, 7, b * 16:b * 16 + 16], ones_b[:, :], p32[bb][:, t * 16:t * 16 + 16], t == 0, t == 1, [b_ones, b_p32[bb]], pb[7])
        c.op("dve", lambda e: e.reciprocal(out=dtM[:, 0:256], in_=PS[:, 7, 0:256]), reads=[pb[7]], writes=[b_dtM])
        c.op("dve", lambda e: e.tensor_tensor(out=AP(omT, NT, [[4 * NTOK, 128], [4, NB], [NTOK, 4], [1, 4]]),
                                              in0=PS[:, 6, 0:256].rearrange("p (b h i) -> p b h i", b=NB, h=4),
                                              in1=dtM[:, 0:256].rearrange("p (b h i) -> p b h i", b=NB, h=4), op=ALU.mult),
             reads=[pb[6], b_dtM], writes=[b_omT])
        c.barrier()
        if stop == "M":
            c.finish("sp")
            return nc

    hT_d = nc.dram_tensor("hT_d", [128, 8, NTOK], BF16, kind="Internal"); b_hTd = c.buf("hTd")
    fa_d = nc.dram_tensor("fa_d", [17, 128, 1024], F32, kind="Internal"); b_fad = c.buf("fad")
    with ExitStack() as ph:
        wG = [sb("wG%d" % i, [128, 8, 3, 128], BF16, ph) for i in range(2)]; b_wG = c.bufs(2, "wG")
        wbA = [sb("wbA%d" % i, [64, 8, 128], BF16, ph) for i in range(2)]; b_wbA = c.bufs(2, "wbA")
        wbB = [sb("wbB%d" % i, [64, 4, 128], BF16, ph) for i in range(2)]; b_wbB = c.bufs(2, "wbB")
        wbM = [sb("wbM%d" % i, [128, 4, 128], BF16, ph) for i in range(2)]; b_wbM = c.bufs(2, "wbM")
        sg = [sb("sg%d" % i, [128, 512], F32, ph) for i in range(3)]; b_sg = c.bufs(3, "sg")
        wv = w_in.rearrange("(k p) n -> p k n", p=128)
        for cc in range(8):
            wi = cc % 2
            for br in range(3):
                c0 = 2560 + 1024 * br + 128 * cc
                c.dma("pool", lambda q: q.dma_start(out=wG[wi][:, :, br, :], in_=wv[:, :, c0:c0 + 128]),
                      writes=[b_wG[wi]], own=b_wG[wi])
            c.dma("pool", lambda q: q.dma_start(out=wbA[wi][:, :, :], in_=w_br_a.rearrange("(h d) n -> d h n", d=64)[:, :, 128 * cc:128 * cc + 128]),
                  writes=[b_wbA[wi]])
            c.dma("pool", lambda q: q.dma_start(out=wbB[wi][:, :, :], in_=w_br_b.rearrange("(h d) n -> d h n", d=64)[:, :, 128 * cc:128 * cc + 128]),
                  writes=[b_wbB[wi]])
            c.dma("pool", lambda q: q.dma_start(out=wbM[wi][:, :, :], in_=w_br_m.rearrange("(h d) n -> d h n", d=128)[:, :, 128 * cc:128 * cc + 128]),
                  writes=[b_wbM[wi]])
            for tg in range(5):
                ntk = 512 if tg < 4 else NS
                tc_ = slice(512 * tg, 512 * tg + ntk)
                for br in range(3):
                    for k in range(8):
                        xr = xT[:, k, 128 + 512 * tg:128 + 512 * tg + 512] if tg < 4 else xsT[:, k, :]
                        MM(PS[:, br, 0:ntk], wG[wi][:, k, br, :], xr, k == 0, k == 7, [b_wG[wi], b_xT, b_xsT], pb[br])
                    c.op("act", lambda e: e.activation(out=sg[br][:, 0:ntk], in_=PS[:, br, 0:ntk], func=AF.Sigmoid),
                         reads=[pb[br]], writes=[b_sg[br]])
                for h in range(8):
                    MM(PS[:, 3, 0:ntk], wbA[wi][:, h, :], oaT[:, h, tc_], h == 0, h == 7, [b_wbA[wi], b_oaT], pb[3])
                for h in range(4):
                    MM(PS[:, 4, 0:ntk], wbB[wi][:, h, :], obT[:, h, tc_], h == 0, h == 3, [b_wbB[wi], b_obT], pb[4])
                for h in range(4):
                    MM(PS[:, 5, 0:ntk], wbM[wi][:, h, :], omT[:, h, tc_], h == 0, h == 3, [b_wbM[wi], b_omT], pb[5])
                for br in range(3):
                    c.op("dve", lambda e: e.tensor_tensor(out=sg[br][:, 0:ntk], in0=sg[br][:, 0:ntk], in1=PS[:, 3 + br, 0:ntk],
                                                          op=ALU.mult), reads=[pb[3 + br], b_sg[br]], writes=[b_sg[br]])
                c.op("dve", lambda e: e.tensor_tensor(out=sg[0][:, 0:ntk], in0=sg[0][:, 0:ntk], in1=sg[1][:, 0:ntk], op=ALU.add),
                     reads=[b_sg[0], b_sg[1]], writes=[b_sg[0]])
                c.op("dve", lambda e: e.tensor_tensor(out=uT[:, cc, tc_], in0=sg[0][:, 0:ntk], in1=sg[2][:, 0:ntk], op=ALU.add),
                     reads=[b_sg[0], b_sg[2]], writes=[b_uT])
        c.barrier()
        if stop == "MERGE":
            c.finish("sp")
            return nc
    c.barrier()
    es2.close()
    NR = 10
    wp = [sb("wp%d" % i, [128, 8, 512], BF16) for i in range(NR)]; b_wp = c.bufs(NR, "wp")
    bdn = [sb("bdn%d" % i, [1, 1024], BF16) for i in range(4)]; b_bdn = c.bufs(4, "bdn")
    piece_slot = {}
    npiece_issued = [0]

    def issue_piece():
        n = npiece_issued[0]
        npiece_issued[0] += 1
        e_, p = n // 6, n % 6
        sl = n % NR
        piece_slot[n] = sl
        if p == 0:
            c.dma("pool", lambda q: q.dma_start(out=bdn[e_ % 4][:, :], in_=b_down[e_:e_ + 1, :]), writes=[b_bdn[e_ % 4]])
        src = w_gu[e_, p] if p < 4 else w_down[e_, p - 4]
        c.dma("pool", lambda q: q.dma_start(out=wp[sl][:, :, :].rearrange("p k n -> p (k n)"), in_=src),
              writes=[b_wp[sl]], own=b_wp[sl])

    for _ in range(min(NR, 6 * n_experts)):
        issue_piece()
    with ExitStack() as ph:
        wo = sb("wo", [128, 8, 1024], BF16, ph); b_wo = c.buf("wo")
        load_w(wo[:, :, :], w_o, 0, 1024, b_wo)
        wr = sb("wr", [128, 8, 32], F32, ph); b_wr = c.buf("wr")
        c.dma("sp", lambda q: q.dma_start(out=wr[:, :, :], in_=w_router.rearrange("(k p) n -> p k n", p=128)), writes=[b_wr])
        lnp = sb("lnp", [128, 2, 1024], F32, ph); brt = sb("brt", [128, 32], F32, ph); b_lnp = c.buf("lnp")
        c.dma("sp", lambda q: q.dma_start(out=lnp[:, 0, :], in_=AP(ln1_g.tensor, 0, [[0, 128], [1, 1024]])), writes=[b_lnp], own=b_lnp)
        c.dma("sp", lambda q: q.dma_start(out=lnp[:, 1, :], in_=AP(ln1_b.tensor, 0, [[0, 128], [1, 1024]])), writes=[b_lnp], own=b_lnp)
        c.dma("sp", lambda q: q.dma_start(out=brt[:, :], in_=AP(b_router.tensor, 0, [[0, 128], [1, 32]])), writes=[b_lnp], own=b_lnp)
        xk = [sb("xk%d" % i, [128, 1024], F32, ph) for i in range(2)]; b_xk = c.bufs(2, "xk")
        zz = [sb("zz%d" % i, [128, 1024], F32, ph) for i in range(2)]; b_zz = c.bufs(2, "zz")
        fa = [sb("fa%d" % i, [128, 1024], F32, ph) for i in range(2)]; b_fa = c.bufs(2, "fa")
        hbs = [sb("hb%d" % i, [128, 1024], BF16, ph) for i in range(4)]; b_hbs = c.bufs(4, "hb")
        hls = [sb("hl%d" % i, [128, 1024], BF16, ph) for i in range(2)]; b_hls = c.bufs(2, "hl")
        hTls = [sb("hTl%d" % i, [128, 8, 128], BF16, ph) for i in range(2)]; b_hTls = c.bufs(2, "hTl")
        wrh = sb("wrh", [128, 8, 32], BF16, ph); wrl = sb("wrl", [128, 8, 32], BF16, ph)
        c.op("dve", lambda e: e.tensor_copy(out=wrh[:, :, :], in_=wr[:, :, :]), reads=[b_wr], writes=[b_wr])
        c.op("dve", lambda e: e.tensor_tensor(out=wrl[:, :, :], in0=wr[:, :, :], in1=wrh[:, :, :], op=ALU.subtract),
             reads=[b_wr], writes=[b_wr])
        hTb = [sb("hTb%d" % i, [128, 8, 128], BF16, ph) for i in range(2)]; b_hTb = c.bufs(2, "hTb")
        st = sb("st", [128, 2, 6], F32, ph); b_st = c.buf("st")
        mv = sb("mvst", [128, 4], F32, ph); b_mv = c.buf("mv")
        lg = sb("lg", [128, 32], F32, ph); b_lg = c.buf("lg")
        m8 = sb("m8", [128, 8], F32, ph); b_m8 = c.buf("m8")
        ix8 = sb("ix8", [128, 8], mybir.dt.uint32, ph); b_ix8 = c.buf("ix8")
        ixf = sb("ixf", [128, 8], F32, ph); b_ixf = c.buf("ixf")
        mskb = sb("mskb", [128, 32], BF16, ph); b_mskb = c.buf("mskb")
        ng = sb("ng", [128, 1], F32, ph); b_ng = c.buf("ng")
        e4 = sb("e4", [128, 4], F32, ph); b_e4 = c.buf("e4")
        ov = sb("ov", [128, 32], F32, ph); b_ov = c.buf("ov")
        dal = sb("dal", [128, 32], F32, ph); b_dal = c.buf("dal")
        d4f = sb("d4f", [128, 4], F32, ph); b_d4f = c.buf("d4f")
        oh4 = sb("oh4", [128, 4, 32], F32, ph); b_oh4 = c.buf("oh4")
        cm = sb("cm", [128, 32], F32, ph); b_cm = c.buf("cm")
        cmb = sb("cmb", [128, 32], BF16, ph); b_cmb = c.buf("cmb")
        c.op("dve", lambda e: e.memset(cm[:, :], 0.0), writes=[b_cm])
        c.op("dve", lambda e: e.memset(cmb[:, :], 0.0), writes=[b_cmb])
        c.op("dve", lambda e: e.memset(mskb[:, :], 0.0), writes=[b_mskb])
        ltri = sb("ltri_s", [128, 128], BF16, ph); b_ltri = c.buf("ltri")
        c.dma("pool", lambda q: q.dma_start(out=ltri[:, :], in_=ltri_d), writes=[b_ltri])
        iot = sb("iot", [128, 32], F32, ph); b_iot = c.buf("iot")
        c.dma("sp", lambda q: q.dma_start(out=iot[:, :], in_=iota_d), writes=[b_iot])
        ecs = sb("ecs_s", [128, 32], F32, ph); b_ecs = c.buf("ecs")
        c.dma("sp", lambda q: q.dma_start(out=ecs[:, :], in_=ecs_d), writes=[b_ecs])
        def P1(tt):
            rows = 128 if tt < 16 else NS
            ti = tt % 2
            tcs = slice(128 * tt, 128 * tt + rows)
            src = xtok[128 * tt:128 * tt + 128, :] if tt < 16 else xs[:, :]
            c.dma("sp", lambda q: q.dma_start(out=xk[ti][0:rows, :], in_=src), writes=[b_xk[ti]])
            yield
            for half in range(2):
                for k in range(8):
                    MM(PS[0:rows, half, :], uT[:, k, tcs], wo[:, k, 512 * half:512 * half + 512], k == 0, k == 7, [b_uT, b_wo], pb[half])
                    yield
                c.op("dve", lambda e: e.scalar_tensor_tensor(out=zz[ti][0:rows, 512 * half:512 * half + 512],
                                                             in0=xk[ti][0:rows, 512 * half:512 * half + 512], scalar=ALPHA,
                                                             in1=PS[0:rows, half, :], op0=ALU.mult, op1=ALU.add),
                     reads=[b_xk[ti], pb[half]], writes=[b_zz[ti]])
                yield
                c.op("dve", lambda e: e.bn_stats(out=st[0:rows, half, :], in_=zz[ti][0:rows, 512 * half:512 * half + 512]),
                     reads=[b_zz[ti]], writes=[b_st])
                yield
            c.op("dve", lambda e: e.bn_aggr(out=mv[0:rows, 0:2], in_=st[0:rows, :, :]), reads=[b_st], writes=[b_mv])
            yield
            c.op("dve", lambda e: e.tensor_scalar(out=mv[0:rows, 2:3], in0=mv[0:rows, 1:2], scalar1=LN_EPS, scalar2=None,
                                                  op0=ALU.add), reads=[b_mv], writes=[b_mv])
            yield
            c.op("act", lambda e: e.activation(out=mv[0:rows, 2:3], in_=mv[0:rows, 2:3], func=AF.Sqrt), reads=[b_mv], writes=[b_mv])
            yield
            c.op("dve", lambda e: e.reciprocal(out=mv[0:rows, 2:3], in_=mv[0:rows, 2:3]), reads=[b_mv], writes=[b_mv])
            yield
            c.op("dve", lambda e: e.scalar_tensor_tensor(out=mv[0:rows, 3:4], in0=mv[0:rows, 0:1], scalar=-1.0, in1=mv[0:rows, 2:3],
                                                         op0=ALU.mult, op1=ALU.mult), reads=[b_mv], writes=[b_mv])
            yield
            c.op("act", lambda e: e.activation(out=zz[ti][0:rows, :], in_=zz[ti][0:rows, :], func=AF.Identity,
                                               scale=mv[0:rows, 2:3], bias=mv[0:rows, 3:4]),
                 reads=[b_zz[ti], b_mv], writes=[b_zz[ti]])
            yield
            c.op("dve", lambda e: e.tensor_tensor(out=zz[ti][0:rows, :], in0=zz[ti][0:rows, :], in1=lnp[0:rows, 0, :], op=ALU.mult),
                 reads=[b_zz[ti], b_lnp], writes=[b_zz[ti]])
            yield
            c.op("dve", lambda e: e.tensor_tensor(out=zz[ti][0:rows, :], in0=zz[ti][0:rows, :], in1=lnp[0:rows, 1, :], op=ALU.add),
                 reads=[b_zz[ti], b_lnp], writes=[b_zz[ti]])
            yield
            c.op("act", lambda e: e.activation(out=fa[ti][0:rows, :], in_=zz[ti][0:rows, :], func=AF.Copy, scale=ALPHA),
                 reads=[b_zz[ti]], writes=[b_fa[ti]])
            yield
            c.dma("sp", lambda q: q.dma_start(out=fa_d.ap()[tt, 0:rows, :], in_=fa[ti][0:rows, :]), reads=[b_fa[ti]],
                  writes=[b_fad], own=b_fad)
            yield
            c.op("act", lambda e: e.activation(out=hbs[tt % 4][0:rows, :], in_=zz[ti][0:rows, :], func=AF.Copy), reads=[b_zz[ti]], writes=[b_hbs[tt % 4]])
            yield
            c.op("dve", lambda e: e.tensor_tensor(out=hls[ti][0:rows, :], in0=zz[ti][0:rows, :], in1=hbs[tt % 4][0:rows, :], op=ALU.subtract),
                 reads=[b_zz[ti], b_hbs[tt % 4]], writes=[b_hls[ti]])
            yield
        def P2(tt):
            rows = 128 if tt < 16 else NS
            ti = tt % 2
            hl, b_hl, hTl, b_hTl = hls[ti], b_hls[ti], hTls[ti], b_hTls[ti]
            for part, (srcb, bsrc) in enumerate(((hbs[tt % 4], b_hbs[tt % 4]), (hl, b_hl))):
                pv = PS[:, 2 + part, :].bitcast(BF16)
                for k in range(8):
                    c.op("pe", lambda e: e.transpose(out=pv[:, 128 * k:128 * k + rows], in_=srcb[0:rows, 128 * k:128 * k + 128],
                                                     identity=ident_b[0:rows, 0:rows]),
                         reads=[bsrc, b_ident], writes=[pb[2 + part]])
                    yield
            pv0 = PS[:, 2, :].bitcast(BF16).rearrange("p (k n) -> p k n", n=128)[:, :, 0:rows]
            pv1 = PS[:, 3, :].bitcast(BF16).rearrange("p (k n) -> p k n", n=128)[:, :, 0:rows]
            c.op("act", lambda e: e.activation(out=hTb[ti][:, :, 0:rows], in_=pv0, func=AF.Copy), reads=[pb[2]], writes=[b_hTb[ti]])
            yield
            c.op("dve", lambda e: e.tensor_copy(out=hTl[:, :, 0:rows], in_=pv1), reads=[pb[3]], writes=[b_hTl])
            yield
            nmm = 0
            for k in range(8):
                for (lt, bl, rt) in ((hTb[ti], b_hTb[ti], wrh), (hTl, b_hTl, wrh), (hTb[ti], b_hTb[ti], wrl)):
                    MM(PS[0:rows, 4, 0:32], lt[:, k, 0:rows], rt[:, k, :], nmm == 0, nmm == 23, [bl, b_wr], pb[4])
                    yield
                    nmm += 1
            c.op("dve", lambda e: e.tensor_tensor(out=lg[0:rows, :], in0=PS[0:rows, 4, 0:32], in1=brt[0:rows, :], op=ALU.add),
                 reads=[pb[4], b_lnp], writes=[b_lg])
            yield
            c.op("dve", lambda e: e.max(out=m8[0:rows, :], in_=lg[0:rows, :]), reads=[b_lg], writes=[b_m8])
            yield
            c.op("dve", lambda e: e.max_index(out=ix8[0:rows, :], in_max=m8[0:rows, :], in_values=lg[0:rows, :]),
                 reads=[b_lg, b_m8], writes=[b_ix8])
            yield
            c.op("dve", lambda e: e.tensor_copy(out=ixf[0:rows, :], in_=ix8[0:rows, :]), reads=[b_ix8], writes=[b_ixf])
            yield
            if tt == 16:
                c.op("dve", lambda e: e.memset(mskb[:, :], 0.0), writes=[b_mskb])
                yield
            c.op("dve", lambda e: e.tensor_scalar(out=mskb[0:rows, :], in0=lg[0:rows, :], scalar1=m8[0:rows, 3:4], scalar2=None,
                                                  op0=ALU.is_ge), reads=[b_lg, b_m8], writes=[b_mskb])
            yield
            c.op("dve", lambda e: e.tensor_scalar(out=ng[0:rows, :], in0=m8[0:rows, 0:1], scalar1=-1.0, scalar2=None, op0=ALU.mult),
                 reads=[b_m8], writes=[b_ng])
            yield
            c.op("act", lambda e: e.activation(out=e4[0:rows, :], in_=m8[0:rows, 0:4], func=AF.Exp, bias=ng[0:rows, 0:1]),
                 reads=[b_m8, b_ng], writes=[b_e4])
            yield
            c.op("dve", lambda e: e.reduce_sum(out=ng[0:rows, :], in_=e4[0:rows, :], axis=AX.X), reads=[b_e4, b_ng], writes=[b_ng])
            yield
            c.op("dve", lambda e: e.reciprocal(out=ng[0:rows, :], in_=ng[0:rows, :]), reads=[b_ng], writes=[b_ng])
            yield
            c.op("dve", lambda e: e.tensor_scalar(out=gates4[0:rows, tt, :], in0=e4[0:rows, :], scalar1=ng[0:rows, 0:1], scalar2=None,
                                                  op0=ALU.mult), reads=[b_e4, b_ng], writes=[b_gates4])
            yield
            MM(PS[0:rows, 5, 0:32], ltri[0:rows, 0:rows], mskb[0:rows, :], True, False, [b_ltri, b_mskb], pb[5])
            yield
            MM(PS[0:rows, 5, 0:32], ones_b[:, 0:rows], cmb[:, :], False, True, [b_ones, b_cmb], pb[5])
            yield
            c.op("dve", lambda e: e.tensor_scalar(out=ov[0:rows, :], in0=PS[0:rows, 5, 0:32], scalar1=float(CAP) - 0.5, scalar2=1.0e6,
                                                  op0=ALU.is_ge, op1=ALU.mult), reads=[pb[5]], writes=[b_ov])
            yield
            c.op("dve", lambda e: e.tensor_tensor(out=dal[0:rows, :], in0=PS[0:rows, 5, 0:32], in1=ecs[0:rows, :], op=ALU.add),
                 reads=[pb[5], b_ecs], writes=[b_dal])
            yield
            c.op("dve", lambda e: e.tensor_tensor(out=dal[0:rows, :], in0=dal[0:rows, :], in1=ov[0:rows, :], op=ALU.add),
                 reads=[b_dal, b_ov], writes=[b_dal])
            yield
            c.op("dve", lambda e: e.tensor_tensor(out=cm[:, :], in0=cm[:, :], in1=mskb[:, :], op=ALU.add), reads=[b_cm, b_mskb], writes=[b_cm])
            yield
            c.op("dve", lambda e: e.tensor_copy(out=cmb[:, :], in_=cm[:, :]), reads=[b_cm], writes=[b_cmb])
            yield
            c.op("dve", lambda e: e.tensor_tensor(out=oh4[0:rows, :, :], in0=iot[0:rows, :].unsqueeze(1).to_broadcast([rows, 4, 32]),
                                                  in1=ixf[0:rows, 0:4].unsqueeze(2).to_broadcast([rows, 4, 32]), op=ALU.is_equal),
                 reads=[b_iot, b_ixf], writes=[b_oh4])
            yield
            c.op("dve", lambda e: e.tensor_tensor(out=oh4[0:rows, :, :], in0=oh4[0:rows, :, :],
                                                  in1=dal[0:rows, :].unsqueeze(1).to_broadcast([rows, 4, 32]), op=ALU.mult),
                 reads=[b_oh4, b_dal], writes=[b_oh4])
            yield
            c.op("dve", lambda e: e.reduce_sum(out=d4f[0:rows, :], in_=oh4[0:rows, :, :], axis=AX.X), reads=[b_oh4], writes=[b_d4f])
            yield
            c.op("dve", lambda e: e.tensor_copy(out=dest4[0:rows, tt, :], in_=d4f[0:rows, :]), reads=[b_d4f], writes=[b_dest4])
            yield
            for k in range(4):
                c.dma("pool", lambda q: q.indirect_dma_start(
                    out=Xd.ap(), out_offset=bass.IndirectOffsetOnAxis(ap=dest4[:, tt, k:k + 1], axis=0),
                    in_=hbs[tt % 4][:, :], in_offset=None, bounds_check=bcreg, oob_is_err=False),
                    reads=[b_hbs[tt % 4], b_dest4], writes=[b_Xd], own=b_Xd)
                yield
        def rr(*gens):
            gens = [g for g in gens if g is not None]
            while gens:
                for g in list(gens):
                    try:
                        next(g)
                    except StopIteration:
                        gens.remove(g)

        rr(P1(0))
        for tt in range(17):
            rr(P1(tt + 1) if tt + 1 < 17 else None, P2(tt))
        c.barrier()
        if stop == "WO":
            c.finish("sp")
            return nc

    with ExitStack() as ph:
        bgu = sb("bgu", [128, 32, 16], F32, ph); b_bgu = c.buf("bgu")
        c.dma("sp", lambda q: q.dma_start(out=bgu[:, :, :], in_=b_gu), writes=[b_bgu])
        bgu7 = sb("bgu7", [128, 32, 8], F32, ph)
        c.op("dve", lambda e: e.tensor_scalar(out=bgu7[:, :, :], in0=bgu[:, :, 8:16], scalar1=7.0, scalar2=None, op0=ALU.add),
             reads=[b_bgu], writes=[b_bgu])
        Xe = [sb("Xe%d" % i, [128, 3, 1024], BF16, ph) for i in range(2)]; b_Xe = c.bufs(2, "Xe")
        XeT = [sb("XeT%d" % i, [128, 8, CAP], BF16, ph) for i in range(2)]; b_XeT = c.bufs(2, "XeT")
        hmid = [sb("hmid%d" % i, [128, 8, CAP], BF16, ph) for i in range(2)]; b_hmid = c.bufs(2, "hmid")
        tg_ = [sb("tgg%d" % i, [128, CAP], F32, ph) for i in range(2)]; b_tg = c.bufs(2, "tgg")
        ts_ = [sb("tss%d" % i, [128, CAP], F32, ph) for i in range(2)]; b_ts = c.bufs(2, "tss")
        tu_ = [sb("tuu%d" % i, [128, CAP], F32, ph) for i in range(2)]; b_tu = c.bufs(2, "tuu")
        Yo = [sb("Yo%d" % i, [128, 1024], F32, ph) for i in range(3)]; b_Yo = c.bufs(3, "Yo")
        nyo = [0]

        def load_X(e_):
            bi = e_ % 2
            c.dma("sp", lambda q: q.dma_start(out=Xe[bi][:, :, :], in_=Xd.ap()[e_ * CAP:(e_ + 1) * CAP, :].rearrange("(b p) n -> p b n", p=128)),
                  reads=[b_Xd], writes=[b_Xe[bi]])

        def emit_T(e_):
            bi = e_ % 2
            for blk in range(3):
                tb = blk % 2
                pv = PS[:, tb, :].bitcast(BF16)
                for k in range(8):
                    c.op("pe", lambda e: e.transpose(out=pv[:, 128 * k:128 * k + 128], in_=Xe[bi][:, blk, 128 * k:128 * k + 128],
                                                     identity=ident_b[:, :]), reads=[b_Xe[bi], b_ident], writes=[pb[tb]])
                EV(XeT[bi][:, :, 128 * blk:128 * blk + 128], pv.rearrange("p (k n) -> p k n", n=128), [pb[tb]], [b_XeT[bi]])

        def stage2(e_, fc):
            bi, ti = e_ % 2, fc % 2
            c.op("dve", lambda e: e.tensor_scalar(out=tu_[ti][:, :], in0=tu_[ti][:, :], scalar1=14.0, scalar2=-6.0,
                                                  op0=ALU.min, op1=ALU.add), reads=[b_tu[ti]], writes=[b_tu[ti]])
            c.op("dve", lambda e: e.tensor_tensor(out=tg_[ti][:, :], in0=tg_[ti][:, :], in1=ts_[ti][:, :], op=ALU.mult),
                 reads=[b_tg[ti], b_ts[ti]], writes=[b_tg[ti]])
            c.op("dve", lambda e: e.tensor_tensor(out=hmid[bi][:, fc, :], in0=tg_[ti][:, :], in1=tu_[ti][:, :], op=ALU.mult),
                 reads=[b_tg[ti], b_tu[ti]], writes=[b_hmid[bi]])

        def emit_GU(e_):
            bi = e_ % 2
            for fc in range(8):
                sl = piece_slot[6 * e_ + fc // 2]
                sub = fc % 2
                gb, ub, ti = 2 + fc % 2, 4 + fc % 2, fc % 2
                for k in range(8):
                    MM(PS[:, gb, 0:CAP], wp[sl][:, k, 128 * sub:128 * sub + 128], XeT[bi][:, k, :], k == 0, k == 7,
                       [b_wp[sl], b_XeT[bi]], pb[gb])
                for k in range(8):
                    MM(PS[:, ub, 0:CAP], wp[sl][:, k, 256 + 128 * sub:256 + 128 * sub + 128], XeT[bi][:, k, :], k == 0, k == 7,
                       [b_wp[sl], b_XeT[bi]], pb[ub])
                if fc >= 1:
                    stage2(e_, fc - 1)
                c.op("dve", lambda e: e.tensor_scalar(out=tg_[ti][:, :], in0=PS[:, gb, 0:CAP], scalar1=bgu[:, e_, fc:fc + 1],
                                                      scalar2=7.0, op0=ALU.add, op1=ALU.min),
                     reads=[pb[gb], b_bgu], writes=[b_tg[ti]])
                c.op("act", lambda e: e.activation(out=ts_[ti][:, :], in_=tg_[ti][:, :], func=AF.Sigmoid, scale=1.702),
                     reads=[b_tg[ti]], writes=[b_ts[ti]])
                c.op("act", lambda e: e.activation(out=tu_[ti][:, :], in_=PS[:, ub, 0:CAP], func=AF.Relu, bias=bgu7[:, e_, fc:fc + 1]),
                     reads=[pb[ub], b_bgu], writes=[b_tu[ti]])
            stage2(e_, 7)

        def emit_D(e_):
            bi = e_ % 2
            for blk in range(3):
                yi = nyo[0] % 3
                nyo[0] += 1
                for half in range(2):
                    db_ = 6 + half
                    sl = piece_slot[6 * e_ + 4 + half]
                    for f in range(8):
                        MM(PS[:, db_, :], hmid[bi][:, f, 128 * blk:128 * blk + 128], wp[sl][:, f, :], f == 0, False,
                           [b_hmid[bi], b_wp[sl]], pb[db_])
                    MM(PS[:, db_, :], ones_b[0:1, :], bdn[e_ % 4][0:1, 512 * half:512 * half + 512], False, True,
                       [b_ones, b_bdn[e_ % 4]], pb[db_])
                    EV(Yo[yi][:, 512 * half:512 * half + 512], PS[:, db_, :], [pb[db_]], [b_Yo[yi]])
                r0 = e_ * CAP + 128 * blk
                c.dma("sp", lambda q: q.dma_start(out=Yd.ap()[r0:r0 + 128, :], in_=Yo[yi][:, :]), reads=[b_Yo[yi]],
                      writes=[b_Yd], own=b_Yd)

        load_X(0)
        emit_T(0)
        for e_ in range(n_experts):
            def top_up(limit):
                while npiece_issued[0] < min(limit, 6 * n_experts):
                    issue_piece()
            if e_ + 1 < n_experts:
                load_X(e_ + 1)
            top_up(6 * e_ + NR)
            emit_GU(e_)
            top_up(6 * e_ + NR + 4)
            if e_ + 1 < n_experts:
                emit_T(e_ + 1)
            emit_D(e_)
            top_up(6 * e_ + NR + 6)
        c.barrier()
    with ExitStack() as ph:
        lnp2 = sb("lnp2", [128, 2, 1024], F32, ph); b_lnp2 = c.buf("lnp2")
        c.dma("sp", lambda q: q.dma_start(out=lnp2[:, 0, :], in_=AP(ln2_g.tensor, 0, [[0, 128], [1, 1024]])), writes=[b_lnp2], own=b_lnp2)
        c.dma("sp", lambda q: q.dma_start(out=lnp2[:, 1, :], in_=AP(ln2_b.tensor, 0, [[0, 128], [1, 1024]])), writes=[b_lnp2], own=b_lnp2)
        Gk = [[sb("Gk%d_%d" % (i, k), [128, 1024], F32, ph) for k in range(4)] for i in range(2)]
        b_Gk = [[c.buf("Gk%d_%d" % (i, k)) for k in range(4)] for i in range(2)]
        fac = [sb("fac%d" % i, [128, 1024], F32, ph) for i in range(2)]; b_fac = c.bufs(2, "fac")
        st2s = [sb("st2_%d" % i, [128, 2, 6], F32, ph) for i in range(2)]; b_st2s = c.bufs(2, "st2")
        mv2s = [sb("mv2_%d" % i, [128, 4], F32, ph) for i in range(2)]; b_mv2s = c.bufs(2, "mv2")
        def CB(tt):
            rows = 128 if tt < 16 else NS
            ti = tt % 2
            c.dma("sp", lambda q: q.dma_start(out=fac[ti][0:rows, :], in_=fa_d.ap()[tt, 0:rows, :]), reads=[b_fad], writes=[b_fac[ti]])
            yield
            for k in range(4):
                c.dma("pool", lambda q: q.indirect_dma_start(
                    out=Gk[ti][k][:, :], out_offset=None, in_=Yd.ap(),
                    in_offset=bass.IndirectOffsetOnAxis(ap=dest4[:, tt, k:k + 1], axis=0),
                    bounds_check=bcreg, oob_is_err=False),
                    reads=[b_Yd, b_dest4], writes=[b_Gk[ti][k]])
                yield
            for k in range(4):
                c.op("dve", lambda e: e.scalar_tensor_tensor(out=fac[ti][0:rows, :], in0=Gk[ti][k][0:rows, :],
                                                             scalar=gates4[0:rows, tt, k:k + 1], in1=fac[ti][0:rows, :],
                                                             op0=ALU.mult, op1=ALU.add),
                     reads=[b_Gk[ti][k], b_gates4, b_fac[ti]], writes=[b_fac[ti]])
                yield
            for half in range(2):
                c.op("dve", lambda e: e.bn_stats(out=st2s[ti][0:rows, half, :], in_=fac[ti][0:rows, 512 * half:512 * half + 512]),
                     reads=[b_fac[ti]], writes=[b_st2s[ti]])
                yield
            c.op("dve", lambda e: e.bn_aggr(out=mv2s[ti][0:rows, 0:2], in_=st2s[ti][0:rows, :, :]), reads=[b_st2s[ti]], writes=[b_mv2s[ti]])
            yield
            c.op("dve", lambda e: e.tensor_scalar(out=mv2s[ti][0:rows, 2:3], in0=mv2s[ti][0:rows, 1:2], scalar1=LN_EPS, scalar2=None,
                                                  op0=ALU.add), reads=[b_mv2s[ti]], writes=[b_mv2s[ti]])
            yield
            c.op("act", lambda e: e.activation(out=mv2s[ti][0:rows, 2:3], in_=mv2s[ti][0:rows, 2:3], func=AF.Sqrt), reads=[b_mv2s[ti]], writes=[b_mv2s[ti]])
            yield
            c.op("dve", lambda e: e.reciprocal(out=mv2s[ti][0:rows, 2:3], in_=mv2s[ti][0:rows, 2:3]), reads=[b_mv2s[ti]], writes=[b_mv2s[ti]])
            yield
            c.op("dve", lambda e: e.scalar_tensor_tensor(out=mv2s[ti][0:rows, 3:4], in0=mv2s[ti][0:rows, 0:1], scalar=-1.0, in1=mv2s[ti][0:rows, 2:3],
                                                         op0=ALU.mult, op1=ALU.mult), reads=[b_mv2s[ti]], writes=[b_mv2s[ti]])
            yield
            c.op("act", lambda e: e.activation(out=fac[ti][0:rows, :], in_=fac[ti][0:rows, :], func=AF.Identity,
                                               scale=mv2s[ti][0:rows, 2:3], bias=mv2s[ti][0:rows, 3:4]),
                 reads=[b_fac[ti], b_mv2s[ti]], writes=[b_fac[ti]])
            yield
            c.op("dve", lambda e: e.tensor_tensor(out=fac[ti][0:rows, :], in0=fac[ti][0:rows, :], in1=lnp2[0:rows, 0, :], op=ALU.mult),
                 reads=[b_fac[ti], b_lnp2], writes=[b_fac[ti]])
            yield
            c.op("dve", lambda e: e.tensor_tensor(out=fac[ti][0:rows, :], in0=fac[ti][0:rows, :], in1=lnp2[0:rows, 1, :], op=ALU.add),
                 reads=[b_fac[ti], b_lnp2], writes=[b_fac[ti]])
            yield
            dst = y_o[128 * tt:128 * tt + 128, :] if tt < 16 else ys_o[:, :]
            c.dma("sp", lambda q: q.dma_start(out=dst, in_=fac[ti][0:rows, :]), reads=[b_fac[ti]], own=b_fac[ti], final=True)
            yield
        def rr2(*gens):
            gens = [g for g in gens if g is not None]
            while gens:
                for g in list(gens):
                    try:
                        next(g)
                    except StopIteration:
                        gens.remove(g)

        for tt in range(0, 17, 2):
            rr2(CB(tt), CB(tt + 1) if tt + 1 < 17 else None)
        c.finish("sp")
    es.close()
    return nc


def _t5_bucket_np(d):
    d = np.maximum(d, 0)
    dl = np.maximum(d, 16).astype(np.float32)
    large = 16 + (np.log(dl / np.float32(16)) / np.float32(math.log(2048 / 16)) * np.float32(16)).astype(np.int32)
    return np.where(d < 16, d, np.minimum(large, 31))


def _make_oh():
    oh = np.zeros((33, 4, FVL), np.float32)
    for s, (dil, wmax) in enumerate(((1, 127), (1, 128), (4, 128), (16, 128))):
        for m in range(FVL):
            dist = m - 127
            if 0 <= dist <= wmax:
                oh[int(_t5_bucket_np(np.array(dist * dil))), s, m] = 1.0
            else:
                oh[32, s, m] = NEG
    return oh


_NC_CACHE = {}
_PREP_ONLY = False


def kernel(x_prompt, x_sample, cache_a_k, cache_a_v, cache_b_k, cache_b_v, cache_mem_k, cache_mem_v,
           mem_prompt, rel_bias, sinks_a, w_in, w_mem_kv, w_br_a, w_br_b, w_br_m, w_o,
           ln1_g, ln1_b, ln2_g, ln2_b, w_router, b_router, w_gu, b_gu, w_down, b_down):
    f32 = np.float32
    A = lambda a: np.ascontiguousarray(np.asarray(a, dtype=f32))
    xp = np.asarray(x_prompt, f32)[0]
    xpad = np.concatenate([np.zeros((2048, 1024), f32), xp], axis=0)
    shared = {
        "memT": A(np.asarray(mem_prompt, f32)[0].T), "rel_bias": A(rel_bias), "sinks": A(sinks_a),
        "oh": _make_oh(), "ident": np.eye(128, dtype=f32),
        "ltri": np.triu(np.ones((128, 128), f32), 1),
        "iota_e": np.tile(np.arange(32, dtype=f32)[None, :], (128, 1)),
        "ecs": np.tile((np.arange(32, dtype=f32) * CAP)[None, :], (128, 1)),
        "w_in": A(np.asarray(w_in)[0]), "w_mem_kv": A(np.asarray(w_mem_kv)[0]), "w_br_a": A(np.asarray(w_br_a)[0]),
        "w_br_b": A(np.asarray(w_br_b)[0]), "w_br_m": A(np.asarray(w_br_m)[0]), "w_o": A(np.asarray(w_o)[0]),
        "ln1_g": A(ln1_g), "ln1_b": A(ln1_b), "ln2_g": A(ln2_g), "ln2_b": A(ln2_b),
        "w_router": A(np.asarray(w_router)[0]), "b_router": A(b_router),
        "w_gu": A(np.asarray(w_gu, f32)[0].reshape(32, 8, 128, 2, 4, 256).transpose(0, 4, 2, 1, 3, 5).reshape(32, 4, 128, 4096)), "b_gu": A(np.asarray(b_gu, f32)[0].reshape(32, 16, 128).transpose(2, 0, 1)),
        "w_down": A(np.asarray(w_down, f32)[0].reshape(32, 8, 128, 2, 512).transpose(0, 3, 2, 1, 4).reshape(32, 2, 128, 4096)), "b_down": A(np.asarray(b_down)[0]),
    }
    cak = np.asarray(cache_a_k, f32)[0]; cavv = np.asarray(cache_a_v, f32)[0]
    cbk = np.asarray(cache_b_k, f32)[0]; cbvv = np.asarray(cache_b_v, f32)[0]
    cmk = np.asarray(cache_mem_k, f32)[0]; cmvv = np.asarray(cache_mem_v, f32)[0]
    xsm = np.asarray(x_sample, f32)
    kk = np.arange(128)
    rows = [1920 + kk]
    for d in (4, 16):
        for i in range(4):
            rows.append(2048 + i - d * (128 - kk))
    rows = np.stack(rows)
    in_maps = []
    for cidx in range(8):
        s0 = 2048 * cidx
        base = s0 + 2048
        i128 = np.arange(128)
        idx0 = np.arange(base - 128, base + 2048)
        idx1 = np.concatenate([base - 512 + 4 * i128 + r for r in range(4)] +
                              [base + 4 * (128 * m + i128) + r for r in range(4) for m in range(4)])
        idx2 = np.concatenate([base - 2048 + 16 * i128 + r for r in range(16)] +
                              [base + 16 * i128 + r for r in range(16)])
        bs = slice(16 * cidx, 16 * cidx + 16)
        m = dict(shared)
        m["xt0"] = A(xpad[idx0].T); m["xt1"] = A(xpad[idx1].T); m["xt2"] = A(xpad[idx2].T)
        m["xtok"] = A(xp[s0:s0 + 2048])
        xs_ = xsm[bs].reshape(64, 1024)
        m["xst"] = A(xs_.T); m["xs"] = A(xs_)
        m["cakT"] = A(cak[bs].transpose(3, 0, 2, 1))
        m["cav"] = A(cavv[bs].transpose(1, 0, 2, 3).reshape(128, 16, 128))
        kb = cbk[bs][:, rows]
        m["cbkT"] = A(kb.transpose(0, 4, 1, 3, 2).reshape(16, 64, 9 * 4 * 128))
        vb = cbvv[bs][:, rows]
        m["cbv"] = A(vb.transpose(0, 2, 1, 3, 4).reshape(16, 128, 9 * 4 * 64))
        m["cmkT"] = A(cmk[bs].transpose(0, 3, 2, 1).reshape(16, 128, 4 * 256))
        m["cmv"] = A(cmvv[bs].reshape(16, 2, 128, 512).transpose(0, 2, 1, 3).reshape(16, 128, 1024))
        m["hmask"] = np.full((128, 1), NEG if cidx == 0 else 0.0, f32)
        in_maps.append(m)
    if _PREP_ONLY:
        return in_maps
    if "nc" not in _NC_CACHE:
        _NC_CACHE["nc"] = build()
    res = run_bass_kernel_spmd(_NC_CACHE["nc"], in_maps, core_ids=list(range(8)))
    R = res.results
    y_prompt = np.concatenate([r["y"] for r in R], axis=0).reshape(1, 16384, 1024)
    y_sample = np.concatenate([r["ys"] for r in R], axis=0).reshape(128, 4, 1024)
    last = R[7]
    a_k = last["ak"].reshape(1, 1, 128, 2, 64); a_v = last["av"].reshape(1, 1, 128, 2, 64)
    b_k = last["bk"].reshape(1, 1, 2048, 4, 64); b_v = last["bv"].reshape(1, 1, 2048, 4, 64)
    m_k = R[0]["mk"].reshape(1, 1, 256, 4, 128); m_v = R[0]["mv"].reshape(1, 1, 256, 4, 128)
    cat = lambda n, w: np.concatenate([r[n] for r in R], axis=0)
    sa_k = cat("sak", 128).reshape(1, 128, 4, 2, 64); sa_v = cat("sav", 128).reshape(1, 128, 4, 2, 64)
    sb_k = cat("sbk", 256).reshape(1, 128, 4, 4, 64); sb_v = cat("sbv", 256).reshape(1, 128, 4, 4, 64)
    return tuple(np.asarray(a, dtype=np.float32) for a in
                 (y_prompt, y_sample, a_k, a_v, b_k, b_v, m_k, m_v, sa_k, sa_v, sb_k, sb_v))
```
